# Optimizing a Trainium2 kernel written in Bass

```python
import math
import jax, jax.numpy as jnp
from jax import lax
import numpy as np

D_MODEL = 1024
BATCH = 4
SEQ = 8192
DEPTH = 2

GDN_HEADS = 8
GDN_HEAD_DIM = 128
GDN_WIDTH = GDN_HEADS * GDN_HEAD_DIM
SSD_HEADS = 16
SSD_HEAD_DIM = 64
SSD_WIDTH = SSD_HEADS * SSD_HEAD_DIM
SSD_GROUPS = 2
SSD_STATE = 128
BC_WIDTH = SSD_GROUPS * SSD_STATE
MIX_WIDTH = GDN_WIDTH + SSD_WIDTH
CONV_K = 7
CHUNK = 64
N_EXPERTS = 16
EXPERT_FF = 512
EC_FACTOR = 2
EPS = 1e-6
DT_MIN = 1e-3
DT_MAX = 0.1

CONV_CH = 3 * GDN_WIDTH + SSD_WIDTH + 2 * BC_WIDTH
GATE_CH = GDN_WIDTH + SSD_WIDTH
SCALAR_CH = 4 * GDN_HEADS + 2 * SSD_HEADS
PROJ_OUT = CONV_CH + GATE_CH + SCALAR_CH

kernel_name = "bidir_hybrid_gdn_ssd_ec_moe"

F32 = jnp.float32


def rms_norm(x, w):
    xf = x.astype(F32)
    y = xf * lax.rsqrt(jnp.mean(xf * xf, axis=-1, keepdims=True) + EPS)
    return (y * w.astype(F32)).astype(x.dtype)


def l2_normalize(t):
    return t * lax.rsqrt(jnp.sum(t * t, axis=-1, keepdims=True) + EPS)


def centred_dwconv(x, w, b):
    pad = (CONV_K - 1) // 2
    L = x.shape[1]
    xp = jnp.pad(x, ((0, 0), (pad, pad), (0, 0)))
    out = xp[:, 0:L] * w[0]
    for tap in range(1, CONV_K):
        out = out + xp[:, tap:tap + L] * w[tap]
    return out + b


def flip_seq(t):
    return jnp.flip(t, axis=1)


def gated_delta_chunked(q, k, v, g, beta):
    Bsz, L, H, Dk = q.shape
    Dv = v.shape[-1]
    N = L // CHUNK

    def to_chunks(t):
        t = t.reshape((Bsz, N, CHUNK) + t.shape[2:])
        return jnp.swapaxes(t, 2, 3)

    q, k, v, g, beta = (to_chunks(t) for t in (q, k, v, g, beta))
    gc = jnp.cumsum(g, axis=-1)
    incl = jnp.tril(jnp.ones((CHUNK, CHUNK), bool))
    strict = jnp.tril(jnp.ones((CHUNK, CHUNK), bool), k=-1)
    decay = jnp.exp(jnp.where(incl, gc[..., :, None] - gc[..., None, :], -jnp.inf))
    kk = jnp.einsum('bnhid,bnhjd->bnhij', k, k)
    A = jnp.where(strict, beta[..., :, None] * kk * decay, 0.0)
    rhs = jnp.concatenate([v * beta[..., None], k * (beta * jnp.exp(gc))[..., None]], axis=-1)
    sol = lax.linalg.triangular_solve(A, rhs, left_side=True, lower=True, unit_diagonal=True)
    u_v, w_k = sol[..., :Dv], sol[..., Dv:]
    qk = jnp.einsum('bnhid,bnhjd->bnhij', q, k) * decay
    q_dec = q * jnp.exp(gc)[..., None]
    g_last = gc[..., -1]
    k_dec = k * jnp.exp(g_last[..., None] - gc)[..., None]

    def step(S, inp):
        u_c, w_c, qk_c, qd_c, kd_c, gl_c = inp
        u_new = u_c - jnp.einsum('bhik,bhkv->bhiv', w_c, S)
        o = jnp.einsum('bhik,bhkv->bhiv', qd_c, S) + jnp.einsum('bhij,bhjv->bhiv', qk_c, u_new)
        S = S * jnp.exp(gl_c)[..., None, None] + jnp.einsum('bhjk,bhjv->bhkv', kd_c, u_new)
        return S, o

    xs = tuple(jnp.moveaxis(t, 1, 0) for t in (u_v, w_k, qk, q_dec, k_dec, g_last))
    S0 = jnp.zeros((Bsz, H, Dk, Dv), F32)
    _, o = lax.scan(step, S0, xs)
    return jnp.transpose(o, (1, 0, 3, 2, 4)).reshape(Bsz, L, H, Dv)


def ssd_chunked(x, dt, A, Bm, Cm):
    Bsz, L, H, P = x.shape
    G, Ns = Bm.shape[2], Bm.shape[3]
    Hg = H // G
    N = L // CHUNK
    x = x.reshape(Bsz, N, CHUNK, G, Hg, P)
    dt = dt.reshape(Bsz, N, CHUNK, G, Hg)
    Bm = Bm.reshape(Bsz, N, CHUNK, G, Ns)
    Cm = Cm.reshape(Bsz, N, CHUNK, G, Ns)
    cs = jnp.cumsum(dt * A.reshape(G, Hg), axis=2)
    incl = jnp.tril(jnp.ones((CHUNK, CHUNK), bool))[:, :, None, None]
    Lmat = jnp.exp(jnp.where(incl, cs[:, :, :, None] - cs[:, :, None, :], -jnp.inf))
    cb = jnp.einsum('bnigs,bnjgs->bnijg', Cm, Bm)
    xdt = x * dt[..., None]
    y_intra = jnp.einsum('bnijgh,bnjghp->bnighp', cb[..., None] * Lmat, xdt)
    cs_last = cs[:, :, -1]
    w_state = jnp.exp(cs_last[:, :, None] - cs)
    chunk_states = jnp.einsum('bnjgs,bnjghp->bnghsp', Bm, xdt * w_state[..., None])

    def step(hst, inp):
        st, dec, c_c, ecs_c = inp
        y = jnp.einsum('bigs,bghsp->bighp', c_c, hst) * ecs_c[..., None]
        hst = hst * jnp.exp(dec)[..., None, None] + st
        return hst, y

    xs = tuple(jnp.moveaxis(t, 1, 0) for t in (chunk_states, cs_last, Cm, jnp.exp(cs)))
    h0 = jnp.zeros((Bsz, G, Hg, Ns, P), F32)
    _, y_inter = lax.scan(step, h0, xs)
    y = y_intra + jnp.moveaxis(y_inter, 0, 1)
    return y.reshape(Bsz, L, H, P)


def hybrid_mixer(n, w_in, conv_w, conv_b, gdn_A_log, gdn_dt_bias, gdn_norm_w,
                 ssd_A_log, ssd_dt_bias, ssd_D, ssd_norm_w, w_out):
    Bsz, L, _ = n.shape
    proj = jnp.einsum('bld,dp->blp', n, w_in)
    conv_in, gates, scal = jnp.split(proj, [CONV_CH, CONV_CH + GATE_CH], axis=-1)
    c = jax.nn.silu(centred_dwconv(conv_in, conv_w, conv_b)).astype(F32)
    q, k, v, xs, Bm, Cm = jnp.split(
        c, [GDN_WIDTH, 2 * GDN_WIDTH, 3 * GDN_WIDTH, 3 * GDN_WIDTH + SSD_WIDTH,
            3 * GDN_WIDTH + SSD_WIDTH + BC_WIDTH], axis=-1)
    g_gate, z = jnp.split(gates.astype(F32), [GDN_WIDTH], axis=-1)
    gh, sh = GDN_HEADS, SSD_HEADS
    b_f, b_b, a_f, a_b, dt_f, dt_b = jnp.split(
        scal.astype(F32), [gh, 2 * gh, 3 * gh, 4 * gh, 4 * gh + sh], axis=-1)

    q = l2_normalize(q.reshape(Bsz, L, gh, GDN_HEAD_DIM)) * (GDN_HEAD_DIM ** -0.5)
    k = l2_normalize(k.reshape(Bsz, L, gh, GDN_HEAD_DIM))
    v = v.reshape(Bsz, L, gh, GDN_HEAD_DIM)
    gA = gdn_A_log.astype(F32)
    gB = gdn_dt_bias.astype(F32)
    g_fw = -jnp.exp(gA[0]) * jax.nn.softplus(a_f + gB[0])
    g_bw = -jnp.exp(gA[1]) * jax.nn.softplus(a_b + gB[1])
    o_fw = gated_delta_chunked(q, k, v, g_fw, jax.nn.sigmoid(b_f))
    o_bw = flip_seq(gated_delta_chunked(flip_seq(q), flip_seq(k), flip_seq(v),
                                        flip_seq(g_bw), flip_seq(jax.nn.sigmoid(b_b))))
    o_gdn = rms_norm(o_fw + o_bw, gdn_norm_w) * jax.nn.silu(g_gate.reshape(Bsz, L, gh, GDN_HEAD_DIM))
    o_gdn = o_gdn.reshape(Bsz, L, GDN_WIDTH)

    x_ssd = xs.reshape(Bsz, L, sh, SSD_HEAD_DIM)
    Bm = Bm.reshape(Bsz, L, SSD_GROUPS, SSD_STATE)
    Cm = Cm.reshape(Bsz, L, SSD_GROUPS, SSD_STATE)
    sA = -jnp.exp(ssd_A_log.astype(F32))
    sB = ssd_dt_bias.astype(F32)
    dt_fw = jax.nn.softplus(dt_f + sB[0])
    dt_bw = jax.nn.softplus(dt_b + sB[1])
    y_fw = ssd_chunked(x_ssd, dt_fw, sA[0], Bm, Cm)
    y_bw = flip_seq(ssd_chunked(flip_seq(x_ssd), flip_seq(dt_bw), sA[1], flip_seq(Bm), flip_seq(Cm)))
    y = y_fw + y_bw + ssd_D.astype(F32)[:, None] * x_ssd
    y = y.reshape(Bsz, L, SSD_WIDTH) * jax.nn.silu(z)
    y = rms_norm(y.reshape(Bsz, L, SSD_GROUPS, SSD_WIDTH // SSD_GROUPS),
                 ssd_norm_w.reshape(SSD_GROUPS, SSD_WIDTH // SSD_GROUPS)).reshape(Bsz, L, SSD_WIDTH)

    mixed = jnp.concatenate([o_gdn, y], axis=-1).astype(n.dtype)
    return jnp.einsum('blm,md->bld', mixed, w_out)


def expert_choice_ffn(n, w_router, w_gate, w_up, w_down):
    Bsz, L, D = n.shape
    cap = EC_FACTOR * L // N_EXPERTS
    logits = jnp.einsum('bld,de->ble', n, w_router).astype(F32)
    aff = jax.nn.softmax(logits, axis=-1)
    vals, idx = lax.top_k(jnp.swapaxes(aff, 1, 2), cap)
    xs = jax.vmap(lambda hb, ib: hb[ib])(n, idx)
    a = jnp.einsum('becd,edf->becf', xs, w_gate)
    u = jnp.einsum('becd,edf->becf', xs, w_up)
    y = jnp.einsum('becf,efd->becd', jax.nn.silu(a) * u, w_down) * vals[..., None].astype(n.dtype)
    return jax.vmap(lambda yb, ib: jnp.zeros((L, D), yb.dtype).at[ib.reshape(-1)].add(yb.reshape(-1, D)))(y, idx)


def _inv_softplus(t):
    return t + jnp.log(-jnp.expm1(-t))


def setup_inputs(seed: int = 0) -> dict:
    key = jax.random.key(seed)
    ks = jax.random.split(key, 20)

    def normal(k, shape, scale):
        return jax.random.normal(k, shape, F32) * scale

    def gain(k, shape):
        return 1.0 + 0.02 * jax.random.normal(k, shape, F32)

    def dt_bias(k, shape):
        u = jax.random.uniform(k, shape, F32)
        dt = jnp.exp(u * (math.log(DT_MAX) - math.log(DT_MIN)) + math.log(DT_MIN))
        return _inv_softplus(dt)

    def a_log(k, shape):
        return jnp.log(jax.random.uniform(k, shape, F32, 1.0, 16.0))

    return {
        "x": jax.random.normal(ks[0], (BATCH, SEQ, D_MODEL), F32),
        "norm1_w": gain(ks[1], (DEPTH, D_MODEL)),
        "w_in": normal(ks[2], (DEPTH, D_MODEL, PROJ_OUT), D_MODEL ** -0.5),
        "conv_w": normal(ks[3], (DEPTH, CONV_K, CONV_CH), CONV_K ** -0.5),
        "conv_b": normal(ks[4], (DEPTH, CONV_CH), 0.01),
        "gdn_A_log": a_log(ks[5], (DEPTH, 2, GDN_HEADS)),
        "gdn_dt_bias": dt_bias(ks[6], (DEPTH, 2, GDN_HEADS)),
        "gdn_norm_w": gain(ks[7], (DEPTH, GDN_HEAD_DIM)),
        "ssd_A_log": a_log(ks[8], (DEPTH, 2, SSD_HEADS)),
        "ssd_dt_bias": dt_bias(ks[9], (DEPTH, 2, SSD_HEADS)),
        "ssd_D": gain(ks[10], (DEPTH, SSD_HEADS)),
        "ssd_norm_w": gain(ks[11], (DEPTH, SSD_WIDTH)),
        "w_out": normal(ks[12], (DEPTH, MIX_WIDTH, D_MODEL), MIX_WIDTH ** -0.5),
        "norm2_w": gain(ks[13], (DEPTH, D_MODEL)),
        "w_router": normal(ks[14], (DEPTH, D_MODEL, N_EXPERTS), D_MODEL ** -0.5),
        "w_gate": normal(ks[15], (DEPTH, N_EXPERTS, D_MODEL, EXPERT_FF), D_MODEL ** -0.5),
        "w_up": normal(ks[16], (DEPTH, N_EXPERTS, D_MODEL, EXPERT_FF), D_MODEL ** -0.5),
        "w_down": normal(ks[17], (DEPTH, N_EXPERTS, EXPERT_FF, D_MODEL), EXPERT_FF ** -0.5),
        "final_norm_w": gain(ks[18], (D_MODEL,)),
    }


def reference(x, norm1_w, w_in, conv_w, conv_b, gdn_A_log, gdn_dt_bias, gdn_norm_w,
              ssd_A_log, ssd_dt_bias, ssd_D, ssd_norm_w, w_out, norm2_w, w_router,
              w_gate, w_up, w_down, final_norm_w):
    h = x
    for layer in range(DEPTH):
        h = h + hybrid_mixer(rms_norm(h, norm1_w[layer]), w_in[layer], conv_w[layer], conv_b[layer],
                             gdn_A_log[layer], gdn_dt_bias[layer], gdn_norm_w[layer],
                             ssd_A_log[layer], ssd_dt_bias[layer], ssd_D[layer], ssd_norm_w[layer],
                             w_out[layer])
        h = h + expert_choice_ffn(rms_norm(h, norm2_w[layer]), w_router[layer], w_gate[layer],
                                  w_up[layer], w_down[layer])
    return rms_norm(h, final_norm_w)
```

```python
import contextlib
import numpy as np
import concourse.bass as bass
import concourse.mybir as mybir
from concourse.bass_utils import run_bass_kernel_spmd

F32 = mybir.dt.float32
F32R = mybir.dt.float32r
BF16 = mybir.dt.bfloat16
I32 = mybir.dt.int32
U32 = mybir.dt.uint32
AF = mybir.ActivationFunctionType
ALU = mybir.AluOpType
AX = mybir.AxisListType

D_MODEL = 1024
SEQ = 8192
BATCH = 4
DEPTH = 2
CH = 64
EPS = 1e-6
NEG = -30000.0
EPOCH = 30000
NDMA_SEMS = 12


class T:
    __slots__ = ("ap", "name", "lw", "rd")

    def __init__(self, ap, name=""):
        self.ap = ap
        self.name = name
        self.lw = None
        self.rd = []

    def __getitem__(self, k):
        return self.ap[k]


class V:
    def __init__(self, parent, ap):
        self.parent = parent
        self.ap = ap

    def __getitem__(self, k):
        return self.ap[k]

    @property
    def lw(self):
        return self.parent.lw

    @lw.setter
    def lw(self, v):
        self.parent.lw = v

    @property
    def rd(self):
        return self.parent.rd

    @rd.setter
    def rd(self, v):
        self.parent.rd = v


def run_rr(gens):
    gens = list(gens)
    while gens:
        for g_ in list(gens):
            try:
                next(g_)
            except StopIteration:
                gens.remove(g_)


class Prog:
    ENG = ("pe", "act", "dve", "pool", "sp")

    def __init__(self, nc, ncols_g, ncols_r):
        self.nc = nc
        self.q = {e: [] for e in self.ENG}
        self.cnt = {e: 0 for e in self.ENG}
        self.dma_i = {e: 0 for e in self.ENG}
        self.dma_last = {}
        self.dma_cnt = {}
        self.last_tok = {}
        self.pending = {e: [] for e in self.ENG}
        self.stack = contextlib.ExitStack()
        self.sems = {}
        self.arena_g = self.stack.enter_context(nc.sbuf_tensor("arena_g", [128, ncols_g], F32))
        self.arena_r = self.stack.enter_context(nc.sbuf_tensor("arena_r", [128, ncols_r], F32R))
        self.ncols = {"g": ncols_g, "r": ncols_r}
        self.ptr = {"g": 0, "r": 0}
        self.banks = [T(self.stack.enter_context(nc.psum_tensor("bank%d" % i, [128, 512], F32)), "bank%d" % i)
                      for i in range(8)]

    def alloc(self, name, parts, free_shape, dt=F32, kind="g"):
        n = int(np.prod(free_shape))
        if dt == BF16:
            cols = (n + 1) // 2
        else:
            cols = n
        cols = (cols + 1) // 2 * 2
        a = self.ptr[kind]
        assert a + cols <= self.ncols[kind], ("SBUF arena overflow", name, kind, a + cols, self.ncols[kind])
        self.ptr[kind] = a + cols
        arena = self.arena_g if kind == "g" else self.arena_r
        if dt == BF16:
            ap = arena[0:parts, a:a + cols].bitcast(BF16)[:, 0:n]
        else:
            ap = arena[0:parts, a:a + n]
        if False:
            pass
        elif kind == "g" and dt != F32:
            ap = ap.bitcast(dt)
        if len(free_shape) == 2:
            ap = ap.rearrange("p (a b) -> p a b", a=free_shape[0])
        elif len(free_shape) == 3:
            ap = ap.rearrange("p (a b c) -> p a b c", a=free_shape[0], b=free_shape[1])
        elif len(free_shape) == 4:
            ap = ap.rearrange("p (a b c d) -> p a b c d", a=free_shape[0], b=free_shape[1], c=free_shape[2])
        return T(ap, name)

    def r(self, name, parts, free_shape):
        return self.alloc(name, parts, free_shape, F32R, "r")

    def mark(self):
        return dict(self.ptr)

    def release(self, mark):
        self.barrier()
        self.ptr = dict(mark)

    def sem(self, key):
        if key not in self.sems:
            nm = "s_" + "_".join(str(k) for k in key)
            self.sems[key] = self.stack.enter_context(self.nc.semaphore(nm))
        return self.sems[key]

    def _deps(self, eng, reads, writes):
        deps = list(self.pending[eng])
        self.pending[eng] = []
        for t in reads:
            if t.lw is not None:
                deps.append(t.lw)
        for t in writes:
            if t.lw is not None:
                deps.append(t.lw)
            deps.extend(t.rd)
        return deps

    def _commit(self, tok, reads, writes):
        self.last_tok[tok[0]] = tok
        for t in reads:
            t.rd.append(tok)
            if len(t.rd) > 48:
                best = {}
                for k, v in t.rd:
                    if best.get(k, -1) < v:
                        best[k] = v
                t.rd = list(best.items())
        for t in writes:
            t.lw = tok
            t.rd = []

    def op(self, eng, fn, reads=(), writes=()):
        deps = self._deps(eng, reads, writes)
        self.cnt[eng] += 1
        n = self.cnt[eng]
        key = ("e", eng, (n - 1) // EPOCH)
        tok = (key, (n - 1) % EPOCH + 1)
        self.q[eng].append((deps, fn, key, 1))
        self._commit(tok, reads, writes)
        return tok

    def dma(self, eng, fn, reads=(), writes=()):
        deps = self._deps(eng, reads, writes)
        i = self.dma_i[eng]
        self.dma_i[eng] += 1
        sk = ("d", eng, i % NDMA_SEMS)
        prev = self.dma_last.get(sk)
        if prev is not None:
            deps.append(prev)
        c = self.dma_cnt.get(sk, 0) + 1
        self.dma_cnt[sk] = c
        tok = (sk, 16 * c)
        self.dma_last[sk] = tok
        self.q[eng].append((deps, fn, sk, 16))
        self._commit(tok, reads, writes)
        return tok

    def barrier(self):
        toks = list(self.last_tok.values())
        for e in self.ENG:
            self.pending[e].extend(toks)

    def mm(self, out, lhsT, rhs, start=True, stop=True, reads=(), writes=()):
        return self.op("pe", lambda e: e.matmul(out, lhsT=lhsT, rhs=rhs, start=start, stop=stop), reads, writes)

    def tr(self, out, in_, ident, reads=(), writes=()):
        return self.op("pe", lambda e: e.transpose(out, in_, ident), reads, writes)

    def act(self, out, in_, func, bias=None, scale=None, accum=None, reads=(), writes=()):
        kw = {}
        if bias is not None:
            kw["bias"] = bias
        if scale is not None:
            kw["scale"] = scale
        if accum is not None:
            kw["accum_out"] = accum
        return self.op("act", lambda e: e.activation(out=out, in_=in_, func=func, **kw), reads, writes)

    def tt(self, out, in0, in1, op, reads=(), writes=(), eng="dve"):
        return self.op(eng, lambda e: e.tensor_tensor(out=out, in0=in0, in1=in1, op=op), reads, writes)

    def ts(self, out, in0, s1, op0, s2=None, op1=None, accum=None, reads=(), writes=(), eng="dve"):
        kw = {}
        if op1 is not None:
            kw["op1"] = op1
        if accum is not None:
            kw["accum_out"] = accum
        return self.op(eng, lambda e: e.tensor_scalar(out=out, in0=in0, scalar1=s1, scalar2=s2, op0=op0, **kw),
                       reads, writes)

    def stt(self, out, in0, scalar, in1, op0, op1, reads=(), writes=()):
        return self.op("dve", lambda e: e.scalar_tensor_tensor(out=out, in0=in0, scalar=scalar, in1=in1,
                                                               op0=op0, op1=op1), reads, writes)

    def copy(self, out, in_, reads=(), writes=(), eng="dve"):
        if eng == "act":
            return self.act(out, in_, AF.Copy, reads=reads, writes=writes)
        return self.op(eng, lambda e: e.tensor_copy(out=out, in_=in_), reads, writes)

    def recip(self, out, in_, reads=(), writes=()):
        return self.op("dve", lambda e: e.reciprocal(out=out, in_=in_), reads, writes)

    def reduce(self, out, in_, op, reads=(), writes=()):
        return self.op("dve", lambda e: e.tensor_reduce(out=out, in_=in_, axis=AX.X, op=op), reads, writes)

    def memset(self, ap, val, writes=(), eng="dve"):
        return self.op(eng, lambda e: e.memset(ap, val), (), writes)

    def ld(self, out, in_, reads=(), writes=(), eng="sp"):
        return self.dma(eng, lambda e: e.dma_start(out=out, in_=in_), reads, writes)

    def emit(self):
        nc = self.nc
        for e in self.ENG:
            for (deps, fn, key, inc) in self.q[e]:
                self.sem(key)
        final = list(self.last_tok.values())
        block = self.stack.enter_context(nc.Block())
        prog = self

        def run(eng_name, eng):
            waited = {}
            for (deps, fn, key, inc) in prog.q[eng_name]:
                need = {}
                for (k, v) in deps:
                    if eng_name == "pe" and k[0] == "e" and k[1] == "pe":
                        continue
                    if waited.get(k, 0) >= v:
                        continue
                    if need.get(k, 0) < v:
                        need[k] = v
                for k, v in need.items():
                    eng.wait_ge(prog.sems[k], v)
                    waited[k] = v
                ins = fn(eng)
                ins.then_inc(prog.sems[key], inc)
            if eng_name == "sp":
                for (k, v) in final:
                    if waited.get(k, 0) < v:
                        eng.wait_ge(prog.sems[k], v)

        @block.tensor
        def _(e):
            run("pe", e)

        @block.scalar
        def _(e):
            run("act", e)

        @block.vector
        def _(e):
            run("dve", e)

        @block.gpsimd
        def _(e):
            run("pool", e)

        @block.sync
        def _(e):
            run("sp", e)

        self.stack.close()


def bc(ap, pos, n):
    shp = list(ap.shape)
    a = ap.unsqueeze(pos)
    shp.insert(pos, n)
    return a.broadcast_to(shp)


def make_consts():
    blocks = {}
    cols = []

    def add(name, arr):
        arr = np.asarray(arr, np.float32).reshape(64, -1)
        blocks[name] = (sum(c.shape[1] for c in cols), arr.shape[1])
        cols.append(arr)

    p = np.arange(64)[:, None]
    i = np.arange(64)[None, :]
    add("ONES64", np.ones((64, 64)))
    add("ONES128", np.ones((64, 128)))
    add("IDENT64", np.eye(64))
    add("ID2", np.concatenate([np.eye(64), np.eye(64)], 1))
    for d in range(2):
        if d == 0:
            tri = (p <= i)
            sl = (p > i)
            negm = np.where(i >= p, 0.0, NEG)
        else:
            tri = (p >= i)
            sl = (p < i)
            negm = np.where(i <= p, 0.0, NEG)
        add("TRI%d" % d, tri)
        add("SL%d" % d, sl)
        add("NEGM%d" % d, negm)
        mk = np.zeros((64, 6, 2, 64), np.float32)
        for l in range(6):
            same = (p >> (l + 1)) == (i >> (l + 1))
            mf = same & (((p >> l) & 1) == 1) & (((i >> l) & 1) == 0)
            m = mf if d == 0 else mf.T
            mk[:, l, 0, :] = m
            mk[:, l, 1, :] = m.T
        add("MK%d" % d, mk)
    return np.concatenate(cols, 1), blocks


CONSTS, CBLK = make_consts()
NK = CONSTS.shape[1]


def load_consts(P, consts_ap, ident_ap):
    craw = P.alloc("craw", 64, [NK])
    P.ld(craw[:], consts_ap[:, :], writes=[craw])
    cr = P.r("cr", 64, [NK])
    P.copy(cr[:], craw[:], reads=[craw], writes=[cr], eng="act")
    ident = P.alloc("ident", 128, [128])
    P.ld(ident[:], ident_ap[:, :], writes=[ident])

    def view(t, name, shape=None):
        o, n = CBLK[name]
        ap = t[:, o:o + n]
        if shape is not None:
            if len(shape) == 2:
                ap = ap.rearrange("p (a b) -> p a b", a=shape[0])
            elif len(shape) == 3:
                ap = ap.rearrange("p (a b c) -> p a b c", a=shape[0], b=shape[1])
        return ap

    return craw, cr, ident, view


def phase_norm_T(P, L, hin, hsum_out, nT_d, ident, n_f32_d=None, w_tile=None):
    na = len(hin)
    mk = P.mark()
    NT = L // 128
    hb = [[P.alloc("hb%d_%d" % (i, j), 128, [1024]) for j in range(na)] for i in range(2)]
    nb = [P.alloc("nb%d" % i, 128, [1024]) for i in range(2)]
    junk = P.alloc("junk", 128, [1024])
    ss = [P.alloc("ss%d" % i, 128, [1]) for i in range(2)]
    rs = [P.alloc("rs%d" % i, 128, [1]) for i in range(2)]
    epst = P.alloc("epst", 128, [1])
    P.memset(epst[:], EPS, writes=[epst])
    nTs = [P.alloc("nTs%d" % i, 128, [8, 512], BF16) for i in range(2)]
    nTv = nT_d.rearrange("(dc p) t -> p dc t", p=128)
    for tt_ in range(NT):
        b = tt_ % 2
        t0 = tt_ * 128
        for j in range(na):
            P.ld(hb[b][j][:], hin[j][t0:t0 + 128, :], writes=[hb[b][j]])
        h = hb[b][0]
        for j in range(1, na):
            P.tt(h[:], h[:], hb[b][j][:], ALU.add, reads=[h, hb[b][j]], writes=[h])
        if hsum_out is not None:
            P.ld(hsum_out[t0:t0 + 128, :], h[:], reads=[h])
        P.act(junk[:], h[:], AF.Square, accum=ss[b][:], reads=[h], writes=[junk, ss[b]])
        P.act(rs[b][:], ss[b][:], AF.Sqrt, bias=epst[:], scale=1.0 / 1024.0, reads=[ss[b], epst], writes=[rs[b]])
        P.recip(rs[b][:], rs[b][:], reads=[rs[b]], writes=[rs[b]])
        P.ts(nb[b][:], h[:], rs[b][:], ALU.mult, reads=[h, rs[b]], writes=[nb[b]])
        if n_f32_d is not None:
            if w_tile is not None:
                P.tt(junk[:], nb[b][:], w_tile[:], ALU.mult, reads=[nb[b], w_tile], writes=[junk])
                P.ld(n_f32_d[t0:t0 + 128, :], junk[:], reads=[junk])
            else:
                P.ld(n_f32_d[t0:t0 + 128, :], nb[b][:], reads=[nb[b]])
        sb_ = (tt_ // 4) % 2
        q4 = tt_ % 4
        for half in range(2):
            bank = P.banks[(tt_ * 2 + half) % 4]
            for k in range(4):
                dc = half * 4 + k
                P.tr(bank[:, k * 128:(k + 1) * 128], nb[b][:, dc * 128:(dc + 1) * 128], ident[:],
                     reads=[nb[b], ident], writes=[bank])
            P.copy(nTs[sb_][:, half * 4:half * 4 + 4, q4 * 128:(q4 + 1) * 128],
                   bank[:].rearrange("p (k t) -> p k t", k=4), reads=[bank], writes=[nTs[sb_]],
                   eng=("act" if half == 0 else "dve"))
        if q4 == 3:
            c0 = (tt_ // 4) * 512
            P.ld(nTv[:, :, c0:c0 + 512], nTs[sb_][:], reads=[nTs[sb_]])
    P.release(mk)


NCOLS_IN = 3360
NCONV = 2304


def phase_inproj(P, L, nT_d, w_in, n1w, cw_ap, cb_ap, C_d, SG_d, SCraw, ident):
    mk = P.mark()
    TB = 512
    NB = L // TB
    WMAX = 1696
    Wb = P.alloc("Wb", 128, [8, WMAX], BF16)
    wst = [P.alloc("wst%d" % i, 128, [848]) for i in range(2)]
    n1 = P.alloc("n1", 128, [8])
    P.ld(n1[:], n1w[:, :], writes=[n1])
    cw = P.alloc("cw", 128, [18, 7])
    cb = P.alloc("cb", 128, [18])
    P.ld(cw[:], cw_ap[:, :, :], writes=[cw])
    P.ld(cb[:], cb_ap[:, :], writes=[cb])
    kld = [0]

    def load_w(base, width):
        pw = width // 2
        for dc in range(8):
            for hf in range(2):
                st = wst[kld[0] % 2]
                P.ld(st[:, 0:pw], w_in[dc * 128:(dc + 1) * 128, base + hf * pw:base + (hf + 1) * pw], writes=[st])
                if kld[0] % 2 == 0:
                    P.ts(Wb[:, dc, hf * pw:(hf + 1) * pw], st[:, 0:pw], n1[:, dc:dc + 1], ALU.mult,
                         reads=[st, n1], writes=[Wb])
                else:
                    P.act(Wb[:, dc, hf * pw:(hf + 1) * pw], st[:, 0:pw], AF.Identity, scale=n1[:, dc:dc + 1],
                          reads=[st, n1], writes=[Wb])
                kld[0] += 1
    nTb = [P.alloc("nTb%d" % i, 128, [8, TB + 6], BF16) for i in range(2)]
    X = [P.alloc("X%d" % i, 128, [TB + 6]) for i in range(2)]
    acc = [P.alloc("acc%d" % i, 128, [TB]) for i in range(2)]
    cT = [P.alloc("cT%d" % i, 128, [TB]) for i in range(2)]
    Cs = [P.alloc("Cs%d" % i, 128, [4, 128]) for i in range(2)]
    SGs = [P.alloc("SGs0", 128, [1024])] * 2
    nTv = nT_d.rearrange("(dc p) t -> p dc t", p=128)
    Cv = C_d.rearrange("(n p) c -> p n c", p=128)
    it = 0
    for wh in range(2):
        base, width, cts = ((0, 1664, range(0, 13)), (1664, 1696, range(13, 18)))[wh]
        load_w(base, width)
        for j in range(NB):
            nb = nTb[j % 2]
            lo = j * TB - 3
            hi = (j + 1) * TB + 3
            if j == 0:
                P.memset(nb[:, :, 0:3], 0.0, writes=[nb])
            if j == NB - 1:
                P.memset(nb[:, :, TB + 3:TB + 6], 0.0, writes=[nb])
            clo = max(lo, 0)
            chi = min(hi, L)
            P.ld(nb[:, :, clo - lo:chi - lo], nTv[:, :, clo:chi], writes=[nb])
            for ct in cts:
                b = it % 2
                it += 1
                bk0 = P.banks[(it * 2) % 4]
                bk1 = P.banks[(it * 2 + 1) % 4]
                for dc in range(8):
                    P.mm(bk0[:, 0:TB], Wb[:, dc, ct * 128 - base:(ct + 1) * 128 - base], nb[:, dc, 0:TB], dc == 0, dc == 7,
                         reads=[Wb, nb], writes=[bk0])
                for dc in range(8):
                    P.mm(bk1[:, 0:6], Wb[:, dc, ct * 128 - base:(ct + 1) * 128 - base], nb[:, dc, TB:TB + 6], dc == 0, dc == 7,
                         reads=[Wb, nb], writes=[bk1])
                P.copy(X[b][:, 0:TB], bk0[:, 0:TB], reads=[bk0], writes=[X[b]], eng="act")
                P.copy(X[b][:, TB:TB + 6], bk1[:, 0:6], reads=[bk1], writes=[X[b]], eng="act")
                P.ts(acc[b][:], X[b][:, 0:TB], cw[:, ct, 0:1], ALU.mult, reads=[X[b], cw], writes=[acc[b]])
                for tap in range(1, 7):
                    P.stt(acc[b][:], X[b][:, tap:tap + TB], cw[:, ct, tap:tap + 1], acc[b][:], ALU.mult, ALU.add,
                          reads=[X[b], cw, acc[b]], writes=[acc[b]])
                P.act(cT[b][:], acc[b][:], AF.Silu, bias=cb[:, ct:ct + 1], reads=[acc[b], cb], writes=[cT[b]])
                bk2 = P.banks[4 + (it % 2)]
                for q in range(4):
                    P.tr(bk2[:, q * 128:(q + 1) * 128], cT[b][:, q * 128:(q + 1) * 128], ident[:],
                         reads=[cT[b], ident], writes=[bk2])
                P.copy(Cs[b][:], bk2[:].rearrange("p (q c) -> p q c", q=4), reads=[bk2], writes=[Cs[b]], eng="act")
                P.ld(Cv[:, j * 4:(j + 1) * 4, ct * 128:(ct + 1) * 128], Cs[b][:], reads=[Cs[b]])
            for q in (range(4) if wh == 1 else []):
                sg = SGs[q % 2]
                for half in range(2):
                    bk = P.banks[6 + half]
                    for dc in range(8):
                        P.mm(bk[:, :], nb[:, dc, 3 + q * 128:3 + (q + 1) * 128],
                             Wb[:, dc, NCONV + half * 512 - base:NCONV + (half + 1) * 512 - base], dc == 0, dc == 7,
                             reads=[Wb, nb], writes=[bk])
                    P.act(sg[:, half * 512:(half + 1) * 512], bk[:, :], AF.Silu, reads=[bk], writes=[sg])
                t0 = j * TB + q * 128
                P.ld(SG_d[t0:t0 + 128, :], sg[:], reads=[sg])
            bk = P.banks[5]
            for cc in (range(TB // 64) if wh == 1 else []):
                for dc in range(8):
                    P.mm(bk[0:64, cc * 32:(cc + 1) * 32], nb[:, dc, 3 + cc * 64:3 + (cc + 1) * 64],
                         Wb[:, dc, 3328 - base:3360 - base], dc == 0, dc == 7, reads=[Wb, nb], writes=[bk])
            nch = TB // 64
            if wh == 1:
                P.copy(SCraw[:, j * nch:(j + 1) * nch, :], bk[0:64, 0:nch * 32].rearrange("p (c s) -> p c s", c=nch),
                       reads=[bk], writes=[SCraw])
    P.release(mk)


def softplus_(P, out, tmp, in_ap, in_reads, scale=1.0):
    P.act(tmp[:], in_ap, AF.Exp, scale=scale, reads=in_reads, writes=[tmp])
    P.act(out[:], tmp[:], AF.Ln, bias=1.0, reads=[tmp], writes=[out])


def gdn_gen(P, L, d, B, C_d, O_d, SCraw, par, ident, cview, cr, craw, zt):
    sfx = "_%d" % d
    NC = L // 64
    N4 = NC * 4
    TRI = cview(cr, "TRI%d" % d)
    SL = cview(cr, "SL%d" % d)
    NEGM = cview(cr, "NEGM%d" % d)
    MK = cview(craw, "MK%d" % d, (6, 2, 64))
    ONES64 = cview(cr, "ONES64")
    ONES128 = cview(cr, "ONES128")
    ID64r = cview(cr, "IDENT64")
    ID64 = cview(craw, "IDENT64")
    ID2 = cview(craw, "ID2", (2, 64))
    lnb = P.alloc("lnb" + sfx, 64, [NC, 4])
    beta = P.alloc("beta" + sfx, 64, [NC, 4])
    g = P.r("g" + sfx, 64, [NC, 4])
    egc = P.alloc("egc" + sfx, 64, [NC, 4])
    bge = P.alloc("bge" + sfx, 64, [NC, 4])
    kdw = P.alloc("kdw" + sfx, 64, [NC, 4])
    edec = P.alloc("edec" + sfx, 128, [NC, 4])
    nexpA = P.alloc("nexpA" + sfx, 64, [4])
    SQ = P.alloc("SQ" + sfx, 64, [8, 128])
    QKn = P.alloc("QKn" + sfx, 64, [8, 128])
    assert NC * 4 <= 512
    sqf = SQ[:].rearrange("p a b -> p (a b)")
    tmpA = V(SQ, sqf[:, 0:NC * 4].rearrange("p (c h) -> p c h", h=4))
    tmpB = V(SQ, sqf[:, 512:512 + NC * 4].rearrange("p (c h) -> p c h", h=4))
    gcs = V(QKn, QKn[:].rearrange("p a b -> p (a b)")[:, 0:NC * 4].rearrange("p (c h) -> p c h", h=4))
    softplus_(P, lnb, tmpA, SCraw[:, :, 4 * d:4 * d + 4], [SCraw], scale=-1.0)
    P.ts(lnb[:], lnb[:], -1.0, ALU.mult, reads=[lnb], writes=[lnb])
    P.act(beta[:], lnb[:], AF.Exp, reads=[lnb], writes=[beta])
    P.tt(tmpB[:], SCraw[:, :, 8 + 4 * d:12 + 4 * d], bc(par[:, 4 * d:4 * d + 4], 1, NC), ALU.add,
         reads=[SCraw, par], writes=[tmpB])
    softplus_(P, tmpB, tmpA, tmpB[:], [tmpB])
    P.act(nexpA[:], par[:, 8 + 4 * d:12 + 4 * d], AF.Exp, reads=[par], writes=[nexpA])
    P.ts(nexpA[:], nexpA[:], -1.0, ALU.mult, reads=[nexpA], writes=[nexpA])
    P.tt(g[:], tmpB[:], bc(nexpA[:], 1, NC), ALU.mult, reads=[tmpB, nexpA], writes=[g])
    b0, b1 = B[0], B[1]
    gflat = g[:].rearrange("p c h -> p (c h)")
    P.mm(b0[0:64, 0:N4], TRI, gflat, reads=[cr, g], writes=[b0])
    P.mm(b1[:, 0:N4], ONES128, gflat, reads=[cr, g], writes=[b1])
    f3 = lambda ap: ap.rearrange("p (c h) -> p c h", h=4)
    P.act(egc[:], f3(b0[0:64, 0:N4]), AF.Exp, reads=[b0], writes=[egc])
    P.act(edec[:], f3(b1[:, 0:N4]), AF.Exp, reads=[b1], writes=[edec])
    P.copy(gcs[:], f3(b0[0:64, 0:N4]), reads=[b0], writes=[gcs], eng="act")
    P.tt(tmpA[:], f3(b1[0:64, 0:N4]), gcs[:], ALU.subtract, reads=[b1, gcs], writes=[tmpA])
    P.act(kdw[:], tmpA[:], AF.Exp, reads=[tmpA], writes=[kdw])
    P.tt(bge[:], beta[:], egc[:], ALU.mult, reads=[beta, egc], writes=[bge])
    QKV = [P.alloc("QKV" + sfx, 64, [3, 4, 128])] * 2
    ss = P.alloc("gss" + sfx, 64, [8])
    rs = P.alloc("grs" + sfx, 64, [8])
    epsq = P.alloc("epsq" + sfx, 64, [1])
    epsk = P.alloc("epsk" + sfx, 64, [1])
    P.memset(epsq[:], 128.0 * EPS, writes=[epsq])
    P.memset(epsk[:], EPS, writes=[epsk])
    Eb = V(SQ, sqf[:, 0:512].rearrange("p (h t j) -> p h t j", h=4, t=2))
    EQ = P.alloc("EQ" + sfx, 64, [4, 64])
    TMP = V(SQ, sqf[:, 512:1024].rearrange("p (h t j) -> p h t j", h=4, t=2))
    UVs = P.alloc("UVs" + sfx, 64, [4, 128])
    OT = P.alloc("OT" + sfx, 64, [4, 128])
    OT2 = [P.alloc("OT2_%d" % i + sfx, 64, [4, 128]) for i in range(2)]
    QKT = P.r("QKT" + sfx, 128, [8, 64])
    GS = P.r("GS" + sfx, 64, [4, 64])
    GT_ = P.r("GT_" + sfx, 64, [4, 64])
    DG = P.r("DG" + sfx, 64, [4, 64])
    AAT = P.r("AAT" + sfx, 64, [4, 2, 64])
    TW = P.r("TW" + sfx, 64, [4, 2, 64])
    YS = P.r("YS" + sfx, 64, [4, 2, 64])
    QKM = P.r("QKM" + sfx, 64, [4, 64])
    BV = P.r("BV" + sfx, 64, [4, 128])
    BGK = P.r("BGK" + sfx, 64, [4, 128])
    KD = P.r("KD" + sfx, 64, [4, 128])
    WKTs = P.r("WKTs" + sfx, 128, [4, 64])
    UN = P.r("UN" + sfx, 64, [4, 128])
    S = P.r("S" + sfx, 128, [4, 128])
    P.copy(S[:], zt[:, 0:512].rearrange("p (h v) -> p h v", h=4), reads=[zt], writes=[S])
    order = list(range(NC)) if d == 0 else list(range(NC - 1, -1, -1))
    for it, ci in enumerate(order):
        t0 = ci * 64
        qkv = QKV[it % 2]
        P.ld(qkv[:].rearrange("p a h v -> p (a h v)"), C_d[t0:t0 + 64, 0:1536], writes=[qkv])
        qk2 = qkv[:, 0:2].rearrange("p a h v -> p (a h) v")
        P.tt(SQ[:], qk2, qk2, ALU.mult, reads=[qkv], writes=[SQ])
        P.reduce(ss[:], SQ[:], ALU.add, reads=[SQ], writes=[ss])
        P.act(rs[:, 0:4], ss[:, 0:4], AF.Sqrt, bias=epsq[:], scale=128.0, reads=[ss, epsq], writes=[rs])
        P.act(rs[:, 4:8], ss[:, 4:8], AF.Sqrt, bias=epsk[:], scale=1.0, reads=[ss, epsk], writes=[rs])
        P.recip(rs[:], rs[:], reads=[rs], writes=[rs])
        P.tt(QKn[:], qk2, bc(rs[:], 2, 128), ALU.mult, reads=[qkv, rs], writes=[QKn])
        for h8 in range(8):
            P.tr(B[0][:, h8 * 64:(h8 + 1) * 64], QKn[:, h8, :], ident[0:64, 0:64], reads=[QKn, ident], writes=[B[0]])
        P.copy(QKT[:], B[0][:].rearrange("p (a i) -> p a i", a=8), reads=[B[0]], writes=[QKT], eng="act")
        yield
        for h in range(4):
            P.mm(B[1][0:64, h * 64:(h + 1) * 64], QKT[:, 4 + h, :], QKT[:, 4 + h, :], reads=[QKT], writes=[B[1]])
        for h in range(4):
            P.mm(B[1][0:64, 256 + h * 64:256 + (h + 1) * 64], QKT[:, 4 + h, :], QKT[:, h, :], reads=[QKT], writes=[B[1]])
        gci = g[:, ci, :]
        P.tt(GS[:], bc(SL, 1, 4), bc(gci, 2, 64), ALU.mult, reads=[cr, g], writes=[GS])
        P.tt(GT_[:], bc(TRI, 1, 4), bc(gci, 2, 64), ALU.mult, reads=[cr, g], writes=[GT_])
        P.tt(DG[:], bc(ID64, 1, 4), bc(lnb[:, ci, :], 2, 64), ALU.mult, reads=[craw, lnb], writes=[DG])
        b2v = B[2][0:64, :].rearrange("p (h t j) -> p h t j", h=4, t=2)
        b3v = B[3][0:64, 0:256].rearrange("p (h j) -> p h j", h=4)
        for h in range(4):
            P.mm(b2v[:, h, 0, :], TRI, GS[:, h, :], True, False, reads=[cr, GS], writes=[B[2]])
            P.mm(b2v[:, h, 0, :], DG[:, h, :], ONES64, False, True, reads=[cr, DG], writes=[B[2]])
            P.mm(b2v[:, h, 1, :], SL, GT_[:, h, :], True, False, reads=[cr, GT_], writes=[B[2]])
            P.mm(b2v[:, h, 1, :], ONES64, DG[:, h, :], False, True, reads=[cr, DG], writes=[B[2]])
            P.mm(b3v[:, h, :], SL, GT_[:, h, :], True, False, reads=[cr, GT_], writes=[B[3]])
            P.mm(b3v[:, h, :], ID64r, NEGM, False, True, reads=[cr], writes=[B[3]])
        P.act(Eb[:], b2v, AF.Exp, reads=[B[2]], writes=[Eb])
        yield
        P.act(EQ[:], b3v, AF.Exp, reads=[B[3]], writes=[EQ])
        kkv = B[1][0:64, 0:256].rearrange("p (h j) -> p h j", h=4)
        P.tt(AAT[:], bc(kkv, 2, 2), Eb[:], ALU.mult, reads=[B[1], Eb], writes=[AAT])
        P.tt(QKM[:], B[1][0:64, 256:512].rearrange("p (h j) -> p h j", h=4), EQ[:], ALU.mult,
             reads=[B[1], EQ], writes=[QKM])
        yield
        P.tt(TMP[:], AAT[:], bc(MK[:, 0], 1, 4), ALU.mult, reads=[AAT, craw], writes=[TMP])
        P.tt(TW[:], bc(ID2, 1, 4), TMP[:], ALU.subtract, reads=[craw, TMP], writes=[TW])
        yield
        b4v = B[0][0:64, :].rearrange("p (h t j) -> p h t j", h=4, t=2)
        b5v = B[1][0:64, :].rearrange("p (h t j) -> p h t j", h=4, t=2)
        for l in range(1, 6):
            for h in range(4):
                P.mm(b4v[:, h, 0, :], AAT[:, h, 1, :], TW[:, h, 0, :], reads=[AAT, TW], writes=[B[0]])
                P.mm(b4v[:, h, 1, :], AAT[:, h, 0, :], TW[:, h, 1, :], reads=[AAT, TW], writes=[B[0]])
            P.tt(YS[:], b4v, bc(MK[:, l], 1, 4), ALU.mult, reads=[B[0], craw], writes=[YS])
            yield
            for h in range(4):
                P.mm(b5v[:, h, 0, :], TW[:, h, 1, :], YS[:, h, 0, :], reads=[TW, YS], writes=[B[1]])
                P.mm(b5v[:, h, 1, :], TW[:, h, 0, :], YS[:, h, 1, :], reads=[TW, YS], writes=[B[1]])
            P.tt(TW[:], TW[:], b5v, ALU.subtract, reads=[TW, B[1]], writes=[TW])
            yield
        P.tt(BV[:], qkv[:, 2], bc(beta[:, ci, :], 2, 128), ALU.mult, reads=[qkv, beta], writes=[BV])
        P.tt(BGK[:], QKn[:, 4:8, :], bc(bge[:, ci, :], 2, 128), ALU.mult, reads=[QKn, bge], writes=[BGK])
        P.tt(KD[:], QKn[:, 4:8, :], bc(kdw[:, ci, :], 2, 128), ALU.mult, reads=[QKn, kdw], writes=[KD])
        for h in range(4):
            P.mm(B[2][0:64, h * 128:(h + 1) * 128], TW[:, h, 1, :], BV[:, h, :], reads=[TW, BV], writes=[B[2]])
        for h in range(4):
            P.mm(B[3][:, 256 + h * 64:256 + (h + 1) * 64], BGK[:, h, :], TW[:, h, 1, :], reads=[TW, BGK], writes=[B[3]])
        P.copy(UVs[:], B[2][0:64, :].rearrange("p (h v) -> p h v", h=4), reads=[B[2]], writes=[UVs], eng="act")
        P.copy(WKTs[:], B[3][:, 256:512].rearrange("p (h i) -> p h i", h=4), reads=[B[3]], writes=[WKTs], eng="act")
        yield
        for h in range(4):
            P.mm(B[0][0:64, h * 128:(h + 1) * 128], WKTs[:, h, :], S[:, h, :], reads=[WKTs, S], writes=[B[0]])
        P.tt(UN[:], UVs[:], B[0][0:64, :].rearrange("p (h v) -> p h v", h=4), ALU.subtract,
             reads=[UVs, B[0]], writes=[UN])
        yield
        for h in range(4):
            P.mm(B[1][0:64, h * 128:(h + 1) * 128], QKT[:, h, :], S[:, h, :], reads=[QKT, S], writes=[B[1]])
        for h in range(4):
            P.mm(B[3][0:64, h * 128:(h + 1) * 128], QKM[:, h, :], UN[:, h, :], reads=[QKM, UN], writes=[B[3]])
        for h in range(4):
            P.mm(B[2][:, h * 128:(h + 1) * 128], KD[:, h, :], UN[:, h, :], reads=[KD, UN], writes=[B[2]])
        P.tt(OT[:], B[1][0:64, :].rearrange("p (h v) -> p h v", h=4), bc(egc[:, ci, :], 2, 128), ALU.mult,
             reads=[B[1], egc], writes=[OT])
        yield
        ot2 = OT2[it % 2]
        P.tt(ot2[:], OT[:], B[3][0:64, :].rearrange("p (h v) -> p h v", h=4), ALU.add,
             reads=[OT, B[3]], writes=[ot2])
        for h in range(4):
            P.stt(S[:, h, :], S[:, h, :], edec[:, ci, h:h + 1], B[2][:, h * 128:(h + 1) * 128], ALU.mult, ALU.add,
                  reads=[S, edec, B[2]], writes=[S])
        P.ld(O_d[t0:t0 + 64, :], ot2[:].rearrange("p h v -> p (h v)"), reads=[ot2])
        yield


def ssd_gen(P, L, d, B, C_d, Y_d, SCraw, par, ident, cview, cr, craw, zt):
    sfx = "_s%d" % d
    NC = L // 64
    TRI = cview(cr, "TRI%d" % d)
    SL = cview(cr, "SL%d" % d)
    NEGM = cview(craw, "NEGM%d" % d)
    ONES128 = cview(cr, "ONES128")
    ID64r = cview(cr, "IDENT64")
    tmpA = P.alloc("stmpA" + sfx, 64, [NC, 8])
    dt_ = P.alloc("dt" + sfx, 64, [NC, 8])
    dtA = P.r("dtA" + sfx, 64, [NC, 8])
    ecs = P.alloc("ecs" + sfx, 64, [NC, 8])
    css = P.alloc("css" + sfx, 64, [NC, 8])
    wst = P.alloc("wst" + sfx, 64, [NC, 8])
    edec = P.alloc("sedec" + sfx, 128, [NC, 8])
    nexpA = P.alloc("snexpA" + sfx, 64, [8])
    NEGM8 = P.r("NEGM8" + sfx, 64, [8, 64])
    P.copy(NEGM8[:], bc(NEGM, 1, 8), reads=[craw], writes=[NEGM8])
    P.tt(dt_[:], SCraw[:, :, 16 + 8 * d:24 + 8 * d], bc(par[:, 16 + 8 * d:24 + 8 * d], 1, NC), ALU.add,
         reads=[SCraw, par], writes=[dt_])
    softplus_(P, dt_, tmpA, dt_[:], [dt_])
    P.act(nexpA[:], par[:, 32 + 8 * d:40 + 8 * d], AF.Exp, reads=[par], writes=[nexpA])
    P.ts(nexpA[:], nexpA[:], -1.0, ALU.mult, reads=[nexpA], writes=[nexpA])
    P.tt(dtA[:], dt_[:], bc(nexpA[:], 1, NC), ALU.mult, reads=[dt_, nexpA], writes=[dtA])
    nh = max(NC // 2, 1)
    parts = [(0, nh), (nh, NC)] if NC >= 2 else [(0, NC)]
    for k, (c0, c1) in enumerate(parts):
        n = (c1 - c0) * 8
        fl = dtA[:, c0:c1, :].rearrange("p c h -> p (c h)")
        f3 = lambda ap: ap.rearrange("p (c h) -> p c h", h=8)
        P.mm(B[k][0:64, 0:n], TRI, fl, reads=[cr, dtA], writes=[B[k]])
        P.mm(B[2 + k][:, 0:n], ONES128, fl, reads=[cr, dtA], writes=[B[2 + k]])
        P.act(ecs[:, c0:c1, :], f3(B[k][0:64, 0:n]), AF.Exp, reads=[B[k]], writes=[ecs])
        P.act(edec[:, c0:c1, :], f3(B[2 + k][:, 0:n]), AF.Exp, reads=[B[2 + k]], writes=[edec])
        P.copy(css[:, c0:c1, :], f3(B[k][0:64, 0:n]), reads=[B[k]], writes=[css], eng="act")
        P.tt(tmpA[:, c0:c1, :], f3(B[2 + k][0:64, 0:n]), css[:, c0:c1, :], ALU.subtract,
             reads=[B[2 + k], css], writes=[tmpA])
    P.act(wst[:], tmpA[:], AF.Exp, reads=[tmpA], writes=[wst])
    XBC = [P.alloc("XBC" + sfx, 64, [768])] * 2
    EL = P.alloc("EL" + sfx, 64, [8, 64])
    YT = P.alloc("YT" + sfx, 64, [8, 64])
    YO = [P.alloc("YO%d" % i + sfx, 64, [8, 64]) for i in range(2)]
    HT = P.alloc("HT" + sfx, 128, [8, 64])
    BCT = P.r("BCT" + sfx, 128, [2, 64])
    Btok = P.r("Btok" + sfx, 64, [128])
    GU = P.r("GU" + sfx, 64, [8, 64])
    GT = P.r("GT" + sfx, 64, [8, 64])
    xdt = P.r("xdt" + sfx, 64, [8, 64])
    xdtw = P.r("xdtw" + sfx, 64, [8, 64])
    HST = P.r("HST" + sfx, 128, [8, 64])
    P.copy(HST[:], zt[:, 0:512].rearrange("p (h q) -> p h q", h=8), reads=[zt], writes=[HST])
    order = list(range(NC)) if d == 0 else list(range(NC - 1, -1, -1))
    fl8 = lambda t: t[:].rearrange("p h i -> p (h i)")
    for it, ci in enumerate(order):
        t0 = ci * 64
        xb = XBC[it % 2]
        P.ld(xb[:], C_d[t0:t0 + 64, 1536:2304], writes=[xb])
        xv = xb[:, 0:512].rearrange("p (h q) -> p h q", h=8)
        P.tr(B[0][:, 0:64], xb[:, 512:640], ident[0:64, 0:64], reads=[xb, ident], writes=[B[0]])
        P.tr(B[0][:, 64:128], xb[:, 640:768], ident[0:64, 0:64], reads=[xb, ident], writes=[B[0]])
        P.copy(BCT[:], B[0][:, 0:128].rearrange("p (a i) -> p a i", a=2), reads=[B[0]], writes=[BCT], eng="act")
        P.copy(Btok[:], xb[:, 512:640], reads=[xb], writes=[Btok], eng="act")
        yield
        P.mm(B[0][0:64, 128:192], BCT[:, 0, :], BCT[:, 1, :], reads=[BCT], writes=[B[0]])
        P.tt(GU[:], bc(TRI, 1, 8), bc(dtA[:, ci, :], 2, 64), ALU.mult, reads=[cr, dtA], writes=[GU])
        P.mm(B[1][0:64, :], SL, fl8(GU), True, False, reads=[cr, GU], writes=[B[1]])
        P.mm(B[1][0:64, :], ID64r, fl8(NEGM8), False, True, reads=[cr, NEGM8], writes=[B[1]])
        P.act(fl8(EL), B[1][0:64, :], AF.Exp, reads=[B[1]], writes=[EL])
        yield
        P.tt(GT[:], EL[:], bc(B[0][0:64, 128:192], 1, 8), ALU.mult, reads=[EL, B[0]], writes=[GT])
        P.tt(xdt[:], xv, bc(dt_[:, ci, :], 2, 64), ALU.mult, reads=[xb, dt_], writes=[xdt])
        P.tt(xdtw[:], xdt[:], bc(wst[:, ci, :], 2, 64), ALU.mult, reads=[xdt, wst], writes=[xdtw])
        yield
        for h in range(8):
            P.mm(B[2][0:64, h * 64:(h + 1) * 64], GT[:, h, :], xdt[:, h, :], reads=[GT, xdt], writes=[B[2]])
        P.mm(B[3][0:64, :], BCT[:, 1, :], fl8(HST), reads=[BCT, HST], writes=[B[3]])
        P.mm(B[1][:, :], Btok[:], fl8(xdtw), reads=[Btok, xdtw], writes=[B[1]])
        v8 = lambda ap: ap.rearrange("p (h q) -> p h q", h=8)
        P.tt(YT[:], v8(B[3][0:64, :]), bc(ecs[:, ci, :], 2, 64), ALU.mult, reads=[B[3], ecs], writes=[YT])
        yield
        yo = YO[it % 2]
        P.tt(yo[:], YT[:], v8(B[2][0:64, :]), ALU.add, reads=[YT, B[2]], writes=[yo])
        P.tt(HT[:], HST[:], bc(edec[:, ci, :], 2, 64), ALU.mult, reads=[HST, edec], writes=[HT])
        P.tt(HST[:], HT[:], v8(B[1][:, :]), ALU.add, reads=[HT, B[1]], writes=[HST])
        P.ld(Y_d[t0:t0 + 64, :], fl8(yo), reads=[yo])
        yield


def phase_outproj(P, L, O_d, Y_d, SG_d, C_d, par_ap, gnw, snw, w_out, P_out, ident):
    mk = P.mark()
    WoB = P.alloc("WoB", 128, [8, 1024], BF16)
    wst = [P.alloc("owst%d" % i, 128, [1024]) for i in range(2)]
    for mc in range(8):
        st = wst[mc % 2]
        P.ld(st[:], w_out[mc * 128:(mc + 1) * 128, :], writes=[st])
        P.copy(WoB[:, mc, :], st[:], reads=[st], writes=[WoB], eng=("act" if mc % 2 else "dve"))
    gnwb = P.alloc("gnwb", 128, [128])
    P.ld(gnwb[:], gnw.partition_broadcast(128), writes=[gnwb])
    snwb = P.alloc("snwb", 128, [512])
    P.ld(snwb[:], snw.partition_broadcast(128), writes=[snwb])
    parb = P.alloc("parb", 128, [64])
    P.ld(parb[:], par_ap.partition_broadcast(128), writes=[parb])
    epsk = P.alloc("oepsk", 128, [1])
    P.memset(epsk[:], EPS, writes=[epsk])
    of_ = [P.alloc("of%d" % i, 128, [4, 128]) for i in range(2)]
    ob_ = [P.alloc("ob%d" % i, 128, [4, 128]) for i in range(2)]
    sg_ = [P.alloc("sg%d" % i, 128, [1024]) for i in range(2)]
    yf_ = [P.alloc("yf%d" % i, 128, [8, 64]) for i in range(2)]
    yb_ = [P.alloc("yb%d" % i, 128, [8, 64]) for i in range(2)]
    xx_ = [P.alloc("xx%d" % i, 128, [8, 64]) for i in range(2)]
    tq = P.alloc("otq", 128, [4, 128])
    ss2 = P.alloc("oss2", 128, [4])
    rs2 = P.alloc("ors2", 128, [4])
    ss3 = P.alloc("oss3", 128, [1])
    rs3 = P.alloc("ors3", 128, [1])
    junk = P.alloc("ojunk", 128, [512])
    mx = [P.alloc("mx%d" % i, 128, [1024]) for i in range(2)]
    mxT = [P.alloc("mxT%d" % i, 128, [8, 128], BF16) for i in range(2)]
    po = [P.alloc("po%d" % i, 128, [1024]) for i in range(2)]
    B = P.banks
    f2 = lambda t: t[:].rearrange("p a b -> p (a b)")
    for tt_ in range(L // 128):
        b = tt_ % 2
        t0 = tt_ * 128
        of, ob, sg, yf, yb, xx, m = of_[b], ob_[b], sg_[b], yf_[b], yb_[b], xx_[b], mx[b]
        P.ld(f2(of), O_d[0][t0:t0 + 128, :], writes=[of])
        P.ld(f2(ob), O_d[1][t0:t0 + 128, :], writes=[ob])
        P.ld(sg[:], SG_d[t0:t0 + 128, :], writes=[sg])
        P.ld(f2(yf), Y_d[0][t0:t0 + 128, :], writes=[yf])
        P.ld(f2(yb), Y_d[1][t0:t0 + 128, :], writes=[yb])
        P.ld(f2(xx), C_d[t0:t0 + 128, 1536:2048], writes=[xx])
        P.tt(of[:], of[:], ob[:], ALU.add, reads=[of, ob], writes=[of])
        P.tt(tq[:], of[:], of[:], ALU.mult, reads=[of], writes=[tq])
        P.reduce(ss2[:], tq[:], ALU.add, reads=[tq], writes=[ss2])
        P.act(rs2[:], ss2[:], AF.Sqrt, bias=epsk[:], scale=1.0 / 128.0, reads=[ss2, epsk], writes=[rs2])
        P.recip(rs2[:], rs2[:], reads=[rs2], writes=[rs2])
        P.tt(of[:], of[:], bc(rs2[:], 2, 128), ALU.mult, reads=[of, rs2], writes=[of])
        sgg = sg[:, 0:512].rearrange("p (h v) -> p h v", h=4)
        P.tt(tq[:], sgg, bc(gnwb[:], 1, 4), ALU.mult, reads=[sg, gnwb], writes=[tq])
        P.tt(m[:, 0:512].rearrange("p (h v) -> p h v", h=4), of[:], tq[:], ALU.mult, reads=[of, tq], writes=[m])
        P.tt(yf[:], yf[:], yb[:], ALU.add, reads=[yf, yb], writes=[yf])
        P.tt(xx[:], xx[:], bc(parb[:, 48:56], 2, 64), ALU.mult, reads=[xx, parb], writes=[xx])
        P.tt(yf[:], yf[:], xx[:], ALU.add, reads=[yf, xx], writes=[yf])
        P.tt(f2(yf), f2(yf), sg[:, 512:1024], ALU.mult, reads=[yf, sg], writes=[yf])
        P.act(junk[:], f2(yf), AF.Square, accum=ss3[:], reads=[yf], writes=[junk, ss3])
        P.act(rs3[:], ss3[:], AF.Sqrt, bias=epsk[:], scale=1.0 / 512.0, reads=[ss3, epsk], writes=[rs3])
        P.recip(rs3[:], rs3[:], reads=[rs3], writes=[rs3])
        P.stt(m[:, 512:1024], f2(yf), rs3[:, 0:1], snwb[:], ALU.mult, ALU.mult, reads=[yf, rs3, snwb], writes=[m])
        for half in range(2):
            bk = B[(tt_ * 2 + half) % 4]
            for k in range(4):
                mc = half * 4 + k
                P.tr(bk[:, k * 128:(k + 1) * 128], m[:, mc * 128:(mc + 1) * 128], ident[:],
                     reads=[m, ident], writes=[bk])
            P.copy(mxT[b][:, half * 4:half * 4 + 4, :], bk[:].rearrange("p (k t) -> p k t", k=4),
                   reads=[bk], writes=[mxT[b]], eng="act")
        for half in range(2):
            bk = B[4 + (tt_ * 2 + half) % 4]
            for mc in range(8):
                P.mm(bk[:, :], mxT[b][:, mc, :], WoB[:, mc, half * 512:(half + 1) * 512], mc == 0, mc == 7,
                     reads=[mxT[b], WoB], writes=[bk])
            P.copy(po[b][:, half * 512:(half + 1) * 512], bk[:, :], reads=[bk], writes=[po[b]], eng="act")
        P.ld(P_out[t0:t0 + 128, :], po[b][:], reads=[po[b]])
    P.release(mk)


def build_mixer(L, na):
    nc = bass.Bass("TRN2", target_bir_lowering=False)
    dtn = nc.dram_tensor
    hin = [dtn("h%d" % i, [L, 1024], F32, kind="ExternalInput").ap() for i in range(na)]
    w_in = dtn("w_in", [1024, NCOLS_IN], F32, kind="ExternalInput").ap()
    cw = dtn("cw", [128, 18, 7], F32, kind="ExternalInput").ap()
    cb = dtn("cb", [128, 18], F32, kind="ExternalInput").ap()
    n1w = dtn("n1w", [128, 8], F32, kind="ExternalInput").ap()
    par_in = dtn("par", [64], F32, kind="ExternalInput").ap()
    gnw = dtn("gnw", [128], F32, kind="ExternalInput").ap()
    snw = dtn("snw", [512], F32, kind="ExternalInput").ap()
    w_out = dtn("w_out", [1024, 1024], F32, kind="ExternalInput").ap()
    consts = dtn("consts", [64, NK], F32, kind="ExternalInput").ap()
    ident_in = dtn("ident", [128, 128], F32, kind="ExternalInput").ap()
    P_out = dtn("P", [L, 1024], F32, kind="ExternalOutput").ap()
    hsum = dtn("hsum", [L, 1024], F32, kind="ExternalOutput").ap() if na > 1 else None
    nT_d = dtn("nT_d", [1024, L], BF16, kind="Internal").ap()
    C_d = dtn("C_d", [L, NCONV], F32, kind="Internal").ap()
    SG_d = dtn("SG_d", [L, 1024], F32, kind="Internal").ap()
    O_d = [dtn("O_d%d" % i, [L, 512], F32, kind="Internal").ap() for i in range(2)]
    Y_d = [dtn("Y_d%d" % i, [L, 512], F32, kind="Internal").ap() for i in range(2)]
    P = Prog(nc, 28250, 15750)
    craw, cr, ident, cview = load_consts(P, consts, ident_in)
    NC = L // 64
    par = P.alloc("par", 64, [64])
    P.ld(par[:], par_in.partition_broadcast(64), writes=[par])
    SCraw = P.alloc("SCraw", 64, [NC, 32])
    zt = P.alloc("zt", 128, [512])
    P.memset(zt[:], 0.0, writes=[zt])
    phase_norm_T(P, L, hin, hsum, nT_d, ident)
    phase_inproj(P, L, nT_d, w_in, n1w, cw, cb, C_d, SG_d, SCraw, ident)
    mk2 = P.mark()
    run_rr([gdn_gen(P, L, d, P.banks[4 * d:4 * d + 4], C_d, O_d[d], SCraw, par, ident, cview, cr, craw, zt)
            for d in range(2)])
    P.release(mk2)
    run_rr([ssd_gen(P, L, d, P.banks[4 * d:4 * d + 4], C_d, Y_d[d], SCraw, par, ident, cview, cr, craw, zt)
            for d in range(2)])
    P.release(mk2)
    phase_outproj(P, L, O_d, Y_d, SG_d, C_d, par_in, gnw, snw, w_out, P_out, ident)
    P.emit()
    return nc


IDENT_NP = np.eye(128, dtype=np.float32)


def mixer_weights(inp, layer, hf):
    w_in = inp["w_in"][layer]
    cols = np.concatenate([
        np.arange(0 + hf * 512, 0 + hf * 512 + 512),
        np.arange(1024 + hf * 512, 1024 + hf * 512 + 512),
        np.arange(2048 + hf * 512, 2048 + hf * 512 + 512),
        np.arange(3072 + hf * 512, 3072 + hf * 512 + 512),
        np.arange(4096 + hf * 128, 4096 + hf * 128 + 128),
        np.arange(4352 + hf * 128, 4352 + hf * 128 + 128),
        np.arange(4608 + hf * 512, 4608 + hf * 512 + 512),
        np.arange(5632 + hf * 512, 5632 + hf * 512 + 512),
        np.arange(6656 + hf * 4, 6656 + hf * 4 + 4),
        np.arange(6664 + hf * 4, 6664 + hf * 4 + 4),
        np.arange(6672 + hf * 4, 6672 + hf * 4 + 4),
        np.arange(6680 + hf * 4, 6680 + hf * 4 + 4),
        np.arange(6688 + hf * 8, 6688 + hf * 8 + 8),
        np.arange(6704 + hf * 8, 6704 + hf * 8 + 8),
    ])
    ccols = cols[:NCONV]
    d = {}
    d["w_in"] = np.ascontiguousarray(w_in[:, cols])
    cwc = inp["conv_w"][layer][:, ccols]
    d["cw"] = np.ascontiguousarray(cwc.T.reshape(18, 128, 7).transpose(1, 0, 2))
    d["cb"] = np.ascontiguousarray(inp["conv_b"][layer][ccols].reshape(18, 128).T)
    d["n1w"] = np.ascontiguousarray(inp["norm1_w"][layer].reshape(8, 128).T)
    par = np.zeros(64, np.float32)
    par[0:8] = inp["gdn_dt_bias"][layer][:, hf * 4:hf * 4 + 4].reshape(-1)
    par[8:16] = inp["gdn_A_log"][layer][:, hf * 4:hf * 4 + 4].reshape(-1)
    par[16:32] = inp["ssd_dt_bias"][layer][:, hf * 8:hf * 8 + 8].reshape(-1)
    par[32:48] = inp["ssd_A_log"][layer][:, hf * 8:hf * 8 + 8].reshape(-1)
    par[48:56] = inp["ssd_D"][layer][hf * 8:hf * 8 + 8]
    d["par"] = par
    d["gnw"] = np.ascontiguousarray(inp["gdn_norm_w"][layer])
    d["snw"] = np.ascontiguousarray(inp["ssd_norm_w"][layer][hf * 512:hf * 512 + 512])
    wo = inp["w_out"][layer]
    d["w_out"] = np.ascontiguousarray(np.concatenate([wo[hf * 512:hf * 512 + 512], wo[1024 + hf * 512:1024 + hf * 512 + 512]], 0))
    d["consts"] = CONSTS
    d["ident"] = IDENT_NP
    return d


def phase_router(P, L, nT_d, w_router, n2w, AFF):
    mk = P.mark()
    TB = 512 if L >= 512 else L
    NQ = TB // 128
    n2 = P.alloc("rn2", 128, [8])
    P.ld(n2[:], n2w[:, :], writes=[n2])
    wr_raw = P.alloc("wr_raw", 128, [8, 16])
    P.ld(wr_raw[:], w_router.rearrange("(dc p) e -> p dc e", p=128), writes=[wr_raw])
    WrB = P.alloc("WrB", 128, [8, 16], BF16)
    P.tt(WrB[:], wr_raw[:], bc(n2[:], 2, 16), ALU.mult, reads=[wr_raw, n2], writes=[WrB])
    nTb = [P.alloc("rnTb%d" % i, 128, [8, TB], BF16) for i in range(2)]
    lgs = P.alloc("lgs", 128, [NQ, 16])
    mx = P.alloc("rmx", 128, [NQ])
    se = P.alloc("rse", 128, [NQ])
    nTv = nT_d.rearrange("(dc p) t -> p dc t", p=128)
    for j in range(L // TB):
        nb = nTb[j % 2]
        P.ld(nb[:], nTv[:, :, j * TB:(j + 1) * TB], writes=[nb])
        bk = P.banks[j % 2]
        for q in range(NQ):
            for dc in range(8):
                P.mm(bk[:, q * 16:(q + 1) * 16], nb[:, dc, q * 128:(q + 1) * 128], WrB[:, dc, :], dc == 0, dc == 7,
                     reads=[nb, WrB], writes=[bk])
        P.copy(lgs[:], bk[:, 0:NQ * 16].rearrange("p (q e) -> p q e", q=NQ), reads=[bk], writes=[lgs], eng="act")
        P.reduce(mx[:], lgs[:], ALU.max, reads=[lgs], writes=[mx])
        P.tt(lgs[:], lgs[:], bc(mx[:], 2, 16), ALU.subtract, reads=[lgs, mx], writes=[lgs])
        P.act(lgs[:], lgs[:], AF.Exp, reads=[lgs], writes=[lgs])
        P.reduce(se[:], lgs[:], ALU.add, reads=[lgs], writes=[se])
        P.recip(se[:], se[:], reads=[se], writes=[se])
        P.tt(AFF[:, j * NQ:(j + 1) * NQ, :], lgs[:], bc(se[:], 2, 16), ALU.mult, reads=[lgs, se], writes=[AFF])
    P.release(mk)


def phase_threshold(P, L, AFF, THRB, cap):
    mk = P.mark()
    NT = L // 128
    ones = P.alloc("tones", 128, [128], BF16)
    P.memset(ones[:], 1.0, writes=[ones])
    gq = P.alloc("gq", 128, [NT, 8], BF16)
    lo = THRB
    hi = P.alloc("hi", 128, [8])
    mid = P.alloc("mid", 128, [8])
    cnt = P.alloc("cnt", 128, [8])
    ge = P.alloc("ge", 128, [8])
    dl = P.alloc("dl", 128, [8])
    P.memset(lo[:], 0.0, writes=[lo])
    P.memset(hi[:], 2.0, writes=[hi])
    bk = P.banks[2]
    NCH = (NT * 8 + 511) // 512
    per = NT // NCH
    for it in range(36):
        P.tt(mid[:], lo[:], hi[:], ALU.add, reads=[lo, hi], writes=[mid])
        P.ts(mid[:], mid[:], 0.5, ALU.mult, reads=[mid], writes=[mid])
        P.tt(gq[:], AFF[:, :, 0:8], bc(mid[:], 1, NT), ALU.is_ge, reads=[AFF, mid], writes=[gq])
        for c in range(NCH):
            P.mm(bk[:, 0:per * 8], ones[:], gq[:, c * per:(c + 1) * per, :].rearrange("p n e -> p (n e)"),
                 c == 0, c == NCH - 1, reads=[ones, gq], writes=[bk])
        P.reduce(cnt[:], bk[:, 0:per * 8].rearrange("p (n e) -> p e n", e=8), ALU.add, reads=[bk], writes=[cnt])
        P.ts(ge[:], cnt[:], float(cap) - 0.5, ALU.is_ge, reads=[cnt], writes=[ge])
        P.tt(dl[:], mid[:], lo[:], ALU.subtract, reads=[mid, lo], writes=[dl])
        P.tt(dl[:], dl[:], ge[:], ALU.mult, reads=[dl, ge], writes=[dl])
        P.tt(lo[:], lo[:], dl[:], ALU.add, reads=[lo, dl], writes=[lo])
        P.tt(dl[:], hi[:], mid[:], ALU.subtract, reads=[hi, mid], writes=[dl])
        P.tt(dl[:], dl[:], ge[:], ALU.mult, reads=[dl, ge], writes=[dl])
        P.tt(hi[:], mid[:], dl[:], ALU.add, reads=[mid, dl], writes=[hi])
    P.release(mk)


def phase_moe_dense(P, L, nT_d, AFF, THRB, n2w, w_gate, w_up, w_down, M_out):
    mk = P.mark()
    SB = 1024 if L >= 1024 else L
    TB = 512
    NSB = L // SB
    NBL = SB // TB
    n2 = P.alloc("mn2", 128, [8])
    P.ld(n2[:], n2w[:, :], writes=[n2])
    acc = P.alloc("macc", 128, [SB // 128, 1024])
    Wg = [P.alloc("Wg%d" % i, 128, [8, 512], BF16) for i in range(2)]
    Wu = [P.alloc("Wu%d" % i, 128, [8, 512], BF16) for i in range(2)]
    Wd = [P.alloc("Wd%d" % i, 128, [4, 1024], BF16) for i in range(2)]
    stg = [P.alloc("stg%d" % i, 128, [2048]) for i in range(2)]
    nTb = [P.alloc("mnTb%d" % i, 128, [8, TB], BF16) for i in range(2)]
    hT = [P.alloc("hT%d" % i, 128, [TB], BF16) for i in range(4)]
    sa = [P.alloc("sa%d" % i, 128, [TB]) for i in range(2)]
    gem = P.alloc("gem", 128, [SB // 128, 8])
    wts = P.alloc("wts", 128, [SB // 128, 8])
    nTv = nT_d.rearrange("(dc p) t -> p dc t", p=128)
    B = P.banks
    kst = 0
    kb = 0
    for sbi in range(NSB):
        s0 = sbi * SB
        affb = AFF[:, s0 // 128:(s0 + SB) // 128, 0:8]
        P.tt(gem[:], affb, bc(THRB[:], 1, SB // 128), ALU.is_ge, reads=[AFF, THRB], writes=[gem])
        P.tt(wts[:], gem[:], affb, ALU.mult, reads=[gem, AFF], writes=[wts])
        for e in range(8):
            wb = (sbi * 8 + e) % 2
            for (src, dst, scaled) in ((w_gate, Wg[wb], True), (w_up, Wu[wb], True)):
                for part in range(2):
                    st = stg[kst % 2]
                    kst += 1
                    P.ld(st[:].rearrange("p (dc f) -> p dc f", dc=4),
                         src[e, part * 512:(part + 1) * 512, :].rearrange("(dc p) f -> p dc f", p=128), writes=[st])
                    for dq in range(4):
                        dcx = part * 4 + dq
                        if dq % 2 == 0:
                            P.act(dst[:, dcx, :], st[:, dq * 512:(dq + 1) * 512], AF.Identity, scale=n2[:, dcx:dcx + 1],
                                  reads=[st, n2], writes=[dst])
                        else:
                            P.ts(dst[:, dcx, :], st[:, dq * 512:(dq + 1) * 512], n2[:, dcx:dcx + 1], ALU.mult,
                                 reads=[st, n2], writes=[dst])
            for part in range(2):
                st = stg[kst % 2]
                kst += 1
                P.ld(st[:].rearrange("p (fc d) -> p fc d", fc=2),
                     w_down[e, part * 256:(part + 1) * 256, :].rearrange("(fc p) d -> p fc d", p=128), writes=[st])
                P.copy(Wd[wb][:, part * 2:(part + 1) * 2, :], st[:].rearrange("p (fc d) -> p fc d", fc=2),
                       reads=[st], writes=[Wd[wb]], eng="act")
            for bl in range(NBL):
                nb = nTb[kb % 2]
                kb += 1
                c0 = s0 + bl * TB
                P.ld(nb[:], nTv[:, :, c0:c0 + TB], writes=[nb])
                for fc in range(4):
                    bg = B[(fc % 2) * 2]
                    bu = B[(fc % 2) * 2 + 1]
                    for dc in range(8):
                        P.mm(bg[:, :], Wg[wb][:, dc, fc * 128:(fc + 1) * 128], nb[:, dc, :], dc == 0, dc == 7,
                             reads=[Wg[wb], nb], writes=[bg])
                    for dc in range(8):
                        P.mm(bu[:, :], Wu[wb][:, dc, fc * 128:(fc + 1) * 128], nb[:, dc, :], dc == 0, dc == 7,
                             reads=[Wu[wb], nb], writes=[bu])
                    s_ = sa[fc % 2]
                    P.act(s_[:], bg[:, :], AF.Silu, reads=[bg], writes=[s_])
                    P.tt(hT[fc][:], s_[:], bu[:, :], ALU.mult, reads=[s_, bu], writes=[hT[fc]])
                for q in range(TB // 128):
                    ti = bl * (TB // 128) + q
                    for half in range(2):
                        by = B[4 + (q * 2 + half) % 4]
                        for fc in range(4):
                            P.mm(by[:, :], hT[fc][:, q * 128:(q + 1) * 128], Wd[wb][:, fc, half * 512:(half + 1) * 512],
                                 fc == 0, fc == 3, reads=[hT[fc], Wd[wb]], writes=[by])
                        av = acc[:, ti, half * 512:(half + 1) * 512]
                        if e == 0:
                            P.act(av, by[:, :], AF.Identity, scale=wts[:, ti, e:e + 1], reads=[by, wts], writes=[acc])
                        else:
                            P.stt(av, by[:, :], wts[:, ti, e:e + 1], av, ALU.mult, ALU.add,
                                  reads=[by, wts, acc], writes=[acc])
        P.ld(M_out[s0:s0 + SB, :].rearrange("(n p) d -> p n d", p=128), acc[:], reads=[acc])
    P.release(mk)


def build_moe(L, na):
    nc = bass.Bass("TRN2", target_bir_lowering=False)
    dtn = nc.dram_tensor
    hin = [dtn("h%d" % i, [L, 1024], F32, kind="ExternalInput").ap() for i in range(na)]
    n2w = dtn("n2w", [128, 8], F32, kind="ExternalInput").ap()
    w_router = dtn("w_router", [1024, 16], F32, kind="ExternalInput").ap()
    w_gate = dtn("w_gate", [8, 1024, 512], F32, kind="ExternalInput").ap()
    w_up = dtn("w_up", [8, 1024, 512], F32, kind="ExternalInput").ap()
    w_down = dtn("w_down", [8, 512, 1024], F32, kind="ExternalInput").ap()
    ident_in = dtn("ident", [128, 128], F32, kind="ExternalInput").ap()
    M_out = dtn("M", [L, 1024], F32, kind="ExternalOutput").ap()
    hsum = dtn("hsum", [L, 1024], F32, kind="ExternalOutput").ap()
    nT_d = dtn("nT_d", [1024, L], BF16, kind="Internal").ap()
    P = Prog(nc, 36000, 16)
    ident = P.alloc("ident", 128, [128])
    P.ld(ident[:], ident_in[:, :], writes=[ident])
    AFF = P.alloc("AFF", 128, [L // 128, 16])
    THRB = P.alloc("THRB", 128, [8])
    phase_norm_T(P, L, hin, hsum, nT_d, ident)
    phase_router(P, L, nT_d, w_router, n2w, AFF)
    phase_threshold(P, L, AFF, THRB, 2 * L // 16)
    phase_moe_dense(P, L, nT_d, AFF, THRB, n2w, w_gate, w_up, w_down, M_out)
    P.emit()
    return nc


BONES_NP = np.kron(np.eye(8, dtype=np.float32), np.ones((16, 16), np.float32))


def moe_weights(inp, layer, hf):
    d = {}
    perm = np.concatenate([np.arange(hf * 8, hf * 8 + 8), np.arange((1 - hf) * 8, (1 - hf) * 8 + 8)])
    d["n2w"] = np.ascontiguousarray(inp["norm2_w"][layer].reshape(8, 128).T)
    d["w_router"] = np.ascontiguousarray(inp["w_router"][layer][:, perm])
    d["w_gate"] = np.ascontiguousarray(inp["w_gate"][layer][hf * 8:hf * 8 + 8])
    d["w_up"] = np.ascontiguousarray(inp["w_up"][layer][hf * 8:hf * 8 + 8])
    d["w_down"] = np.ascontiguousarray(inp["w_down"][layer][hf * 8:hf * 8 + 8])
    d["ident"] = IDENT_NP
    return d


def build_final(L, na):
    nc = bass.Bass("TRN2", target_bir_lowering=False)
    dtn = nc.dram_tensor
    hin = [dtn("h%d" % i, [L, 1024], F32, kind="ExternalInput").ap() for i in range(na)]
    fw = dtn("fw", [1024], F32, kind="ExternalInput").ap()
    out = dtn("out", [L, 1024], F32, kind="ExternalOutput").ap()
    P = Prog(nc, 20000, 16)
    fwb = P.alloc("fwb", 128, [1024])
    P.ld(fwb[:], fw.partition_broadcast(128), writes=[fwb])
    hb = [[P.alloc("hb%d_%d" % (i, j), 128, [1024]) for j in range(na)] for i in range(2)]
    junk = P.alloc("junk", 128, [1024])
    ob = [P.alloc("ob%d" % i, 128, [1024]) for i in range(2)]
    ss = [P.alloc("ss%d" % i, 128, [1]) for i in range(2)]
    rs = [P.alloc("rs%d" % i, 128, [1]) for i in range(2)]
    epst = P.alloc("epst", 128, [1])
    P.memset(epst[:], EPS, writes=[epst])
    for tt_ in range(L // 128):
        b = tt_ % 2
        t0 = tt_ * 128
        for j in range(na):
            P.ld(hb[b][j][:], hin[j][t0:t0 + 128, :], writes=[hb[b][j]])
        h = hb[b][0]
        for j in range(1, na):
            P.tt(h[:], h[:], hb[b][j][:], ALU.add, reads=[h, hb[b][j]], writes=[h])
        P.act(junk[:], h[:], AF.Square, accum=ss[b][:], reads=[h], writes=[junk, ss[b]])
        P.act(rs[b][:], ss[b][:], AF.Sqrt, bias=epst[:], scale=1.0 / 1024.0, reads=[ss[b], epst], writes=[rs[b]])
        P.recip(rs[b][:], rs[b][:], reads=[rs[b]], writes=[rs[b]])
        P.stt(ob[b][:], h[:], rs[b][:, 0:1], fwb[:], ALU.mult, ALU.mult, reads=[h, rs[b], fwb], writes=[ob[b]])
        P.ld(out[t0:t0 + 128, :], ob[b][:], reads=[ob[b]])
    P.emit()
    return nc


_CACHE = {}


def _get(kind, L, na):
    key = (kind, L, na)
    if key not in _CACHE:
        _CACHE[key] = {"mixer": build_mixer, "moe": build_moe, "final": build_final}[kind](L, na)
    return _CACHE[key]


def _run(nc, maps, cores, tag):
    import time, sys
    t = time.time()
    r = run_bass_kernel_spmd(nc, maps, core_ids=cores).results
    print("[kernel] launch %s: %.1fs" % (tag, time.time() - t), file=sys.stderr, flush=True)
    return r


def kernel(**inp):
    inp = {k: np.asarray(v) for k, v in inp.items()}
    x = inp["x"]
    Bn, L, D = x.shape
    cores = list(range(8))
    zeros = np.zeros((L, D), np.float32)
    add = [[np.ascontiguousarray(x[b]), zeros, zeros] for b in range(Bn)]
    for layer in range(DEPTH):
        nc = _get("mixer", L, 3)
        maps = []
        for c in cores:
            b, hf = c // 2, c % 2
            d = mixer_weights(inp, layer, hf)
            for i in range(3):
                d["h%d" % i] = add[b][i]
            maps.append(d)
        res = _run(nc, maps, cores, 'mixer%d' % layer)
        add = [[res[2 * b]["hsum"], res[2 * b]["P"], res[2 * b + 1]["P"]] for b in range(Bn)]
        nc = _get("moe", L, 3)
        maps = []
        for c in cores:
            b, hf = c // 2, c % 2
            d = moe_weights(inp, layer, hf)
            for i in range(3):
                d["h%d" % i] = add[b][i]
            maps.append(d)
        res = _run(nc, maps, cores, 'moe%d' % layer)
        add = [[res[2 * b]["hsum"], res[2 * b]["M"], res[2 * b + 1]["M"]] for b in range(Bn)]
    nc = _get("final", L // 2, 3)
    maps = []
    for c in cores:
        b, hf = c // 2, c % 2
        d = {"fw": np.ascontiguousarray(inp["final_norm_w"])}
        for i in range(3):
            d["h%d" % i] = np.ascontiguousarray(add[b][i][hf * (L // 2):(hf + 1) * (L // 2)])
        maps.append(d)
    res = _run(nc, maps, cores, 'final')
    out = np.stack([np.concatenate([res[2 * b]["out"], res[2 * b + 1]["out"]], 0) for b in range(Bn)], 0)
    return out.astype(np.float32)
```

```python
import contextlib
import numpy as np
import concourse.bass as bass
import concourse.mybir as mybir
from concourse.bass_utils import run_bass_kernel_spmd

F32 = mybir.dt.float32
F32R = mybir.dt.float32r
BF16 = mybir.dt.bfloat16
I32 = mybir.dt.int32
U32 = mybir.dt.uint32
AF = mybir.ActivationFunctionType
ALU = mybir.AluOpType
AX = mybir.AxisListType

D_MODEL = 1024
SEQ = 8192
BATCH = 4
DEPTH = 2
CH = 64
EPS = 1e-6
NEG = -30000.0
EPOCH = 30000
NDMA_SEMS = 12
SAME_ENGINE_SYNC = True


class T:
    __slots__ = ("ap", "name", "lw", "rd")

    def __init__(self, ap, name=""):
        self.ap = ap
        self.name = name
        self.lw = None
        self.rd = []

    def __getitem__(self, k):
        return self.ap[k]


class V:
    def __init__(self, parent, ap):
        self.parent = parent
        self.ap = ap

    def __getitem__(self, k):
        return self.ap[k]

    @property
    def lw(self):
        return self.parent.lw

    @lw.setter
    def lw(self, v):
        self.parent.lw = v

    @property
    def rd(self):
        return self.parent.rd

    @rd.setter
    def rd(self, v):
        self.parent.rd = v


def run_rr(gens):
    gens = list(gens)
    while gens:
        for g_ in list(gens):
            try:
                next(g_)
            except StopIteration:
                gens.remove(g_)


class Prog:
    ENG = ("pe", "act", "dve", "pool", "sp")

    def __init__(self, nc, ncols_g, ncols_r):
        self.nc = nc
        self.q = {e: [] for e in self.ENG}
        self.cnt = {e: 0 for e in self.ENG}
        self.dma_i = {e: 0 for e in self.ENG}
        self.dma_last = {}
        self.dma_cnt = {}
        self.last_tok = {}
        self.pending = {e: [] for e in self.ENG}
        self.stack = contextlib.ExitStack()
        self.sems = {}
        self.arena_g = self.stack.enter_context(nc.sbuf_tensor("arena_g", [128, ncols_g], F32))
        self.arena_r = self.stack.enter_context(nc.sbuf_tensor("arena_r", [128, ncols_r], F32R))
        self.ncols = {"g": ncols_g, "r": ncols_r}
        self.ptr = {"g": 0, "r": 0}
        self.banks = [T(self.stack.enter_context(nc.psum_tensor("bank%d" % i, [128, 512], F32)), "bank%d" % i)
                      for i in range(8)]

    def alloc(self, name, parts, free_shape, dt=F32, kind="g"):
        n = int(np.prod(free_shape))
        if dt == BF16:
            cols = (n + 1) // 2
        else:
            cols = n
        cols = (cols + 1) // 2 * 2
        a = self.ptr[kind]
        assert a + cols <= self.ncols[kind], ("SBUF arena overflow", name, kind, a + cols, self.ncols[kind])
        self.ptr[kind] = a + cols
        arena = self.arena_g if kind == "g" else self.arena_r
        if dt == BF16:
            ap = arena[0:parts, a:a + cols].bitcast(BF16)[:, 0:n]
        else:
            ap = arena[0:parts, a:a + n]
        if False:
            pass
        elif kind == "g" and dt != F32:
            ap = ap.bitcast(dt)
        if len(free_shape) == 2:
            ap = ap.rearrange("p (a b) -> p a b", a=free_shape[0])
        elif len(free_shape) == 3:
            ap = ap.rearrange("p (a b c) -> p a b c", a=free_shape[0], b=free_shape[1])
        elif len(free_shape) == 4:
            ap = ap.rearrange("p (a b c d) -> p a b c d", a=free_shape[0], b=free_shape[1], c=free_shape[2])
        return T(ap, name)

    def r(self, name, parts, free_shape):
        return self.alloc(name, parts, free_shape, F32R, "r")

    def mark(self):
        return dict(self.ptr)

    def release(self, mark):
        self.barrier()
        self.ptr = dict(mark)

    def sem(self, key):
        if key not in self.sems:
            nm = "s_" + "_".join(str(k) for k in key)
            self.sems[key] = self.stack.enter_context(self.nc.semaphore(nm))
        return self.sems[key]

    def _deps(self, eng, reads, writes):
        deps = list(self.pending[eng])
        self.pending[eng] = []
        for t in reads:
            if t.lw is not None:
                deps.append(t.lw)
        for t in writes:
            if t.lw is not None:
                deps.append(t.lw)
            deps.extend(t.rd)
        return deps

    def _commit(self, tok, reads, writes):
        self.last_tok[tok[0]] = tok
        for t in reads:
            t.rd.append(tok)
            if len(t.rd) > 48:
                best = {}
                for k, v in t.rd:
                    if best.get(k, -1) < v:
                        best[k] = v
                t.rd = list(best.items())
        for t in writes:
            t.lw = tok
            t.rd = []

    def op(self, eng, fn, reads=(), writes=()):
        deps = self._deps(eng, reads, writes)
        self.cnt[eng] += 1
        n = self.cnt[eng]
        key = ("e", eng, (n - 1) // EPOCH)
        tok = (key, (n - 1) % EPOCH + 1)
        self.q[eng].append((deps, fn, key, 1))
        self._commit(tok, reads, writes)
        return tok

    def dma(self, eng, fn, reads=(), writes=()):
        deps = self._deps(eng, reads, writes)
        i = self.dma_i[eng]
        self.dma_i[eng] += 1
        sk = ("d", eng, i % NDMA_SEMS)
        prev = self.dma_last.get(sk)
        if prev is not None:
            deps.append(prev)
        c = self.dma_cnt.get(sk, 0) + 1
        self.dma_cnt[sk] = c
        tok = (sk, 16 * c)
        self.dma_last[sk] = tok
        self.q[eng].append((deps, fn, sk, 16))
        self._commit(tok, reads, writes)
        return tok

    def barrier(self):
        toks = list(self.last_tok.values())
        for e in self.ENG:
            self.pending[e].extend(toks)

    def mm(self, out, lhsT, rhs, start=True, stop=True, reads=(), writes=()):
        return self.op("pe", lambda e: e.matmul(out, lhsT=lhsT, rhs=rhs, start=start, stop=stop), reads, writes)

    def tr(self, out, in_, ident, reads=(), writes=()):
        return self.op("pe", lambda e: e.transpose(out, in_, ident), reads, writes)

    def act(self, out, in_, func, bias=None, scale=None, accum=None, reads=(), writes=()):
        kw = {}
        if bias is not None:
            kw["bias"] = bias
        if scale is not None:
            kw["scale"] = scale
        if accum is not None:
            kw["accum_out"] = accum
        return self.op("act", lambda e: e.activation(out=out, in_=in_, func=func, **kw), reads, writes)

    def tt(self, out, in0, in1, op, reads=(), writes=(), eng="dve"):
        return self.op(eng, lambda e: e.tensor_tensor(out=out, in0=in0, in1=in1, op=op), reads, writes)

    def ts(self, out, in0, s1, op0, s2=None, op1=None, accum=None, reads=(), writes=(), eng="dve"):
        kw = {}
        if op1 is not None:
            kw["op1"] = op1
        if accum is not None:
            kw["accum_out"] = accum
        return self.op(eng, lambda e: e.tensor_scalar(out=out, in0=in0, scalar1=s1, scalar2=s2, op0=op0, **kw),
                       reads, writes)

    def stt(self, out, in0, scalar, in1, op0, op1, reads=(), writes=()):
        return self.op("dve", lambda e: e.scalar_tensor_tensor(out=out, in0=in0, scalar=scalar, in1=in1,
                                                               op0=op0, op1=op1), reads, writes)

    def copy(self, out, in_, reads=(), writes=(), eng="dve"):
        if eng == "act":
            return self.act(out, in_, AF.Copy, reads=reads, writes=writes)
        return self.op(eng, lambda e: e.tensor_copy(out=out, in_=in_), reads, writes)

    def recip(self, out, in_, reads=(), writes=()):
        return self.op("dve", lambda e: e.reciprocal(out=out, in_=in_), reads, writes)

    def reduce(self, out, in_, op, reads=(), writes=()):
        return self.op("dve", lambda e: e.tensor_reduce(out=out, in_=in_, axis=AX.X, op=op), reads, writes)

    def memset(self, ap, val, writes=(), eng="dve"):
        return self.op(eng, lambda e: e.memset(ap, val), (), writes)

    def ld(self, out, in_, reads=(), writes=(), eng="sp"):
        return self.dma(eng, lambda e: e.dma_start(out=out, in_=in_), reads, writes)

    def emit(self):
        nc = self.nc
        for e in self.ENG:
            for (deps, fn, key, inc) in self.q[e]:
                self.sem(key)
        final = list(self.last_tok.values())
        block = self.stack.enter_context(nc.Block())
        prog = self

        def run(eng_name, eng):
            waited = {}
            for (deps, fn, key, inc) in prog.q[eng_name]:
                need = {}
                for (k, v) in deps:
                    if eng_name == "pe" and k[0] == "e" and k[1] == "pe":
                        continue
                    if not SAME_ENGINE_SYNC and k[0] == "e" and k[1] == eng_name:
                        continue
                    if waited.get(k, 0) >= v:
                        continue
                    if need.get(k, 0) < v:
                        need[k] = v
                for k, v in need.items():
                    eng.wait_ge(prog.sems[k], v)
                    waited[k] = v
                ins = fn(eng)
                ins.then_inc(prog.sems[key], inc)
            if eng_name == "sp":
                for (k, v) in final:
                    if waited.get(k, 0) < v:
                        eng.wait_ge(prog.sems[k], v)

        @block.tensor
        def _(e):
            run("pe", e)

        @block.scalar
        def _(e):
            run("act", e)

        @block.vector
        def _(e):
            run("dve", e)

        @block.gpsimd
        def _(e):
            run("pool", e)

        @block.sync
        def _(e):
            run("sp", e)

        self.stack.close()


def bc(ap, pos, n):
    shp = list(ap.shape)
    a = ap.unsqueeze(pos)
    shp.insert(pos, n)
    return a.broadcast_to(shp)


def make_consts():
    blocks = {}
    cols = []

    def add(name, arr):
        arr = np.asarray(arr, np.float32).reshape(64, -1)
        blocks[name] = (sum(c.shape[1] for c in cols), arr.shape[1])
        cols.append(arr)

    p = np.arange(64)[:, None]
    i = np.arange(64)[None, :]
    add("ONES64", np.ones((64, 64)))
    add("ONES128", np.ones((64, 128)))
    add("IDENT64", np.eye(64))
    add("ID2", np.concatenate([np.eye(64), np.eye(64)], 1))
    for d in range(2):
        if d == 0:
            tri = (p <= i)
            sl = (p > i)
            negm = np.where(i >= p, 0.0, NEG)
        else:
            tri = (p >= i)
            sl = (p < i)
            negm = np.where(i <= p, 0.0, NEG)
        add("TRI%d" % d, tri)
        add("SL%d" % d, sl)
        add("NEGM%d" % d, negm)
        mk = np.zeros((64, 6, 2, 64), np.float32)
        for l in range(6):
            same = (p >> (l + 1)) == (i >> (l + 1))
            mf = same & (((p >> l) & 1) == 1) & (((i >> l) & 1) == 0)
            m = mf if d == 0 else mf.T
            mk[:, l, 0, :] = m
            mk[:, l, 1, :] = m.T
        add("MK%d" % d, mk)
    return np.concatenate(cols, 1), blocks


CONSTS, CBLK = make_consts()
NK = CONSTS.shape[1]


def load_consts(P, consts_ap, ident_ap):
    craw = P.alloc("craw", 64, [NK])
    P.ld(craw[:], consts_ap[:, :], writes=[craw])
    cr = P.r("cr", 64, [NK])
    P.copy(cr[:], craw[:], reads=[craw], writes=[cr], eng="act")
    ident = P.alloc("ident", 128, [128])
    P.ld(ident[:], ident_ap[:, :], writes=[ident])

    def view(t, name, shape=None):
        o, n = CBLK[name]
        ap = t[:, o:o + n]
        if shape is not None:
            if len(shape) == 2:
                ap = ap.rearrange("p (a b) -> p a b", a=shape[0])
            elif len(shape) == 3:
                ap = ap.rearrange("p (a b c) -> p a b c", a=shape[0], b=shape[1])
        return ap

    return craw, cr, ident, view


def phase_norm_T(P, L, hin, hsum_out, nT_d, ident, n_f32_d=None, w_tile=None):
    na = len(hin)
    mk = P.mark()
    NT = L // 128
    hb = [[P.alloc("hb%d_%d" % (i, j), 128, [1024]) for j in range(na)] for i in range(2)]
    nb = [P.alloc("nb%d" % i, 128, [1024]) for i in range(2)]
    junk = P.alloc("junk", 128, [1024])
    ss = [P.alloc("ss%d" % i, 128, [1]) for i in range(2)]
    rs = [P.alloc("rs%d" % i, 128, [1]) for i in range(2)]
    epst = P.alloc("epst", 128, [1])
    P.memset(epst[:], EPS, writes=[epst])
    nTs = [P.alloc("nTs%d" % i, 128, [8, 512], BF16) for i in range(2)]
    nTv = nT_d.rearrange("(dc p) t -> p dc t", p=128)
    for tt_ in range(NT):
        b = tt_ % 2
        t0 = tt_ * 128
        for j in range(na):
            P.ld(hb[b][j][:], hin[j][t0:t0 + 128, :], writes=[hb[b][j]])
        h = hb[b][0]
        for j in range(1, na):
            P.tt(h[:], h[:], hb[b][j][:], ALU.add, reads=[h, hb[b][j]], writes=[h])
        if hsum_out is not None:
            P.ld(hsum_out[t0:t0 + 128, :], h[:], reads=[h])
        P.act(junk[:], h[:], AF.Square, accum=ss[b][:], reads=[h], writes=[junk, ss[b]])
        P.act(rs[b][:], ss[b][:], AF.Sqrt, bias=epst[:], scale=1.0 / 1024.0, reads=[ss[b], epst], writes=[rs[b]])
        P.recip(rs[b][:], rs[b][:], reads=[rs[b]], writes=[rs[b]])
        P.ts(nb[b][:], h[:], rs[b][:], ALU.mult, reads=[h, rs[b]], writes=[nb[b]])
        if n_f32_d is not None:
            if w_tile is not None:
                P.tt(junk[:], nb[b][:], w_tile[:], ALU.mult, reads=[nb[b], w_tile], writes=[junk])
                P.ld(n_f32_d[t0:t0 + 128, :], junk[:], reads=[junk])
            else:
                P.ld(n_f32_d[t0:t0 + 128, :], nb[b][:], reads=[nb[b]])
        sb_ = (tt_ // 4) % 2
        q4 = tt_ % 4
        for half in range(2):
            bank = P.banks[(tt_ * 2 + half) % 4]
            for k in range(4):
                dc = half * 4 + k
                P.tr(bank[:, k * 128:(k + 1) * 128], nb[b][:, dc * 128:(dc + 1) * 128], ident[:],
                     reads=[nb[b], ident], writes=[bank])
            P.copy(nTs[sb_][:, half * 4:half * 4 + 4, q4 * 128:(q4 + 1) * 128],
                   bank[:].rearrange("p (k t) -> p k t", k=4), reads=[bank], writes=[nTs[sb_]],
                   eng=("act" if half == 0 else "dve"))
        if q4 == 3:
            c0 = (tt_ // 4) * 512
            P.ld(nTv[:, :, c0:c0 + 512], nTs[sb_][:], reads=[nTs[sb_]])
    P.release(mk)


NCOLS_IN = 3360
NCONV = 2304


def phase_inproj(P, L, nT_d, w_in, n1w, cw_ap, cb_ap, C_d, SG_d, SCraw, ident):
    mk = P.mark()
    TB = 512
    NB = L // TB
    WMAX = 1696
    Wb = P.alloc("Wb", 128, [8, WMAX], BF16)
    wst = [P.alloc("wst%d" % i, 128, [848]) for i in range(2)]
    n1 = P.alloc("n1", 128, [8])
    P.ld(n1[:], n1w[:, :], writes=[n1])
    cw = P.alloc("cw", 128, [18, 7])
    cb = P.alloc("cb", 128, [18])
    P.ld(cw[:], cw_ap[:, :, :], writes=[cw])
    P.ld(cb[:], cb_ap[:, :], writes=[cb])
    kld = [0]

    def load_w(base, width):
        pw = width // 2
        for dc in range(8):
            for hf in range(2):
                st = wst[kld[0] % 2]
                P.ld(st[:, 0:pw], w_in[dc * 128:(dc + 1) * 128, base + hf * pw:base + (hf + 1) * pw], writes=[st])
                if kld[0] % 2 == 0:
                    P.ts(Wb[:, dc, hf * pw:(hf + 1) * pw], st[:, 0:pw], n1[:, dc:dc + 1], ALU.mult,
                         reads=[st, n1], writes=[Wb])
                else:
                    P.act(Wb[:, dc, hf * pw:(hf + 1) * pw], st[:, 0:pw], AF.Identity, scale=n1[:, dc:dc + 1],
                          reads=[st, n1], writes=[Wb])
                kld[0] += 1
    nTb = [P.alloc("nTb%d" % i, 128, [8, TB + 6], BF16) for i in range(2)]
    X = [P.alloc("X%d" % i, 128, [TB + 6]) for i in range(2)]
    acc = [P.alloc("acc%d" % i, 128, [TB]) for i in range(2)]
    cT = [P.alloc("cT%d" % i, 128, [TB]) for i in range(2)]
    Cs = [P.alloc("Cs%d" % i, 128, [4, 128]) for i in range(2)]
    SGs = [P.alloc("SGs0", 128, [1024])] * 2
    nTv = nT_d.rearrange("(dc p) t -> p dc t", p=128)
    Cv = C_d.rearrange("(n p) c -> p n c", p=128)
    it = 0
    for wh in range(2):
        base, width, cts = ((0, 1664, range(0, 13)), (1664, 1696, range(13, 18)))[wh]
        load_w(base, width)
        for j in range(NB):
            nb = nTb[j % 2]
            lo = j * TB - 3
            hi = (j + 1) * TB + 3
            if j == 0:
                P.memset(nb[:, :, 0:3], 0.0, writes=[nb])
            if j == NB - 1:
                P.memset(nb[:, :, TB + 3:TB + 6], 0.0, writes=[nb])
            clo = max(lo, 0)
            chi = min(hi, L)
            P.ld(nb[:, :, clo - lo:chi - lo], nTv[:, :, clo:chi], writes=[nb])
            for ct in cts:
                b = it % 2
                it += 1
                bk0 = P.banks[(it * 2) % 4]
                bk1 = P.banks[(it * 2 + 1) % 4]
                for dc in range(8):
                    P.mm(bk0[:, 0:TB], Wb[:, dc, ct * 128 - base:(ct + 1) * 128 - base], nb[:, dc, 0:TB], dc == 0, dc == 7,
                         reads=[Wb, nb], writes=[bk0])
                for dc in range(8):
                    P.mm(bk1[:, 0:6], Wb[:, dc, ct * 128 - base:(ct + 1) * 128 - base], nb[:, dc, TB:TB + 6], dc == 0, dc == 7,
                         reads=[Wb, nb], writes=[bk1])
                P.copy(X[b][:, 0:TB], bk0[:, 0:TB], reads=[bk0], writes=[X[b]], eng="act")
                P.copy(X[b][:, TB:TB + 6], bk1[:, 0:6], reads=[bk1], writes=[X[b]], eng="act")
                P.ts(acc[b][:], X[b][:, 0:TB], cw[:, ct, 0:1], ALU.mult, reads=[X[b], cw], writes=[acc[b]])
                for tap in range(1, 7):
                    P.stt(acc[b][:], X[b][:, tap:tap + TB], cw[:, ct, tap:tap + 1], acc[b][:], ALU.mult, ALU.add,
                          reads=[X[b], cw, acc[b]], writes=[acc[b]])
                P.act(cT[b][:], acc[b][:], AF.Silu, bias=cb[:, ct:ct + 1], reads=[acc[b], cb], writes=[cT[b]])
                bk2 = P.banks[4 + (it % 2)]
                for q in range(4):
                    P.tr(bk2[:, q * 128:(q + 1) * 128], cT[b][:, q * 128:(q + 1) * 128], ident[:],
                         reads=[cT[b], ident], writes=[bk2])
                P.copy(Cs[b][:], bk2[:].rearrange("p (q c) -> p q c", q=4), reads=[bk2], writes=[Cs[b]], eng="act")
                P.ld(Cv[:, j * 4:(j + 1) * 4, ct * 128:(ct + 1) * 128], Cs[b][:], reads=[Cs[b]])
            for q in (range(4) if wh == 1 else []):
                sg = SGs[q % 2]
                for half in range(2):
                    bk = P.banks[6 + half]
                    for dc in range(8):
                        P.mm(bk[:, :], nb[:, dc, 3 + q * 128:3 + (q + 1) * 128],
                             Wb[:, dc, NCONV + half * 512 - base:NCONV + (half + 1) * 512 - base], dc == 0, dc == 7,
                             reads=[Wb, nb], writes=[bk])
                    P.act(sg[:, half * 512:(half + 1) * 512], bk[:, :], AF.Silu, reads=[bk], writes=[sg])
                t0 = j * TB + q * 128
                P.ld(SG_d[t0:t0 + 128, :], sg[:], reads=[sg])
            bk = P.banks[5]
            for cc in (range(TB // 64) if wh == 1 else []):
                for dc in range(8):
                    P.mm(bk[0:64, cc * 32:(cc + 1) * 32], nb[:, dc, 3 + cc * 64:3 + (cc + 1) * 64],
                         Wb[:, dc, 3328 - base:3360 - base], dc == 0, dc == 7, reads=[Wb, nb], writes=[bk])
            nch = TB // 64
            if wh == 1:
                P.copy(SCraw[:, j * nch:(j + 1) * nch, :], bk[0:64, 0:nch * 32].rearrange("p (c s) -> p c s", c=nch),
                       reads=[bk], writes=[SCraw])
    P.release(mk)


def softplus_(P, out, tmp, in_ap, in_reads, scale=1.0):
    P.act(tmp[:], in_ap, AF.Exp, scale=scale, reads=in_reads, writes=[tmp])
    P.act(out[:], tmp[:], AF.Ln, bias=1.0, reads=[tmp], writes=[out])


def gdn_gen(P, L, d, B, C_d, O_d, SCraw, par, ident, cview, cr, craw, zt):
    sfx = "_%d" % d
    b0h = [B[0], B[0]]
    b1h = [B[1], B[1]]
    NC = L // 64
    N4 = NC * 4
    TRI = cview(cr, "TRI%d" % d)
    SL = cview(cr, "SL%d" % d)
    NEGM = cview(cr, "NEGM%d" % d)
    MK = cview(craw, "MK%d" % d, (6, 2, 64))
    ONES64 = cview(cr, "ONES64")
    ONES128 = cview(cr, "ONES128")
    ID64r = cview(cr, "IDENT64")
    ID64 = cview(craw, "IDENT64")
    ID2 = cview(craw, "ID2", (2, 64))
    lnb = P.alloc("lnb" + sfx, 64, [NC, 4])
    beta = P.alloc("beta" + sfx, 64, [NC, 4])
    g = P.r("g" + sfx, 64, [NC, 4])
    egc = P.alloc("egc" + sfx, 64, [NC, 4])
    bge = P.alloc("bge" + sfx, 64, [NC, 4])
    kdw = P.alloc("kdw" + sfx, 64, [NC, 4])
    edec = P.alloc("edec" + sfx, 128, [NC, 4])
    nexpA = P.alloc("nexpA" + sfx, 64, [4])
    SQ = P.alloc("SQ" + sfx, 64, [8, 128])
    QKn = P.alloc("QKn" + sfx, 64, [8, 128])
    assert NC * 4 <= 512
    sqf = SQ[:].rearrange("p a b -> p (a b)")
    tmpA = V(SQ, sqf[:, 0:NC * 4].rearrange("p (c h) -> p c h", h=4))
    tmpB = V(SQ, sqf[:, 512:512 + NC * 4].rearrange("p (c h) -> p c h", h=4))
    gcs = V(QKn, QKn[:].rearrange("p a b -> p (a b)")[:, 0:NC * 4].rearrange("p (c h) -> p c h", h=4))
    softplus_(P, lnb, tmpA, SCraw[:, :, 4 * d:4 * d + 4], [SCraw], scale=-1.0)
    P.ts(lnb[:], lnb[:], -1.0, ALU.mult, reads=[lnb], writes=[lnb])
    P.act(beta[:], lnb[:], AF.Exp, reads=[lnb], writes=[beta])
    P.tt(tmpB[:], SCraw[:, :, 8 + 4 * d:12 + 4 * d], bc(par[:, 4 * d:4 * d + 4], 1, NC), ALU.add,
         reads=[SCraw, par], writes=[tmpB])
    softplus_(P, tmpB, tmpA, tmpB[:], [tmpB])
    P.act(nexpA[:], par[:, 8 + 4 * d:12 + 4 * d], AF.Exp, reads=[par], writes=[nexpA])
    P.ts(nexpA[:], nexpA[:], -1.0, ALU.mult, reads=[nexpA], writes=[nexpA])
    P.tt(g[:], tmpB[:], bc(nexpA[:], 1, NC), ALU.mult, reads=[tmpB, nexpA], writes=[g])
    b0, b1 = B[0], B[1]
    gflat = g[:].rearrange("p c h -> p (c h)")
    P.mm(b0[0:64, 0:N4], TRI, gflat, reads=[cr, g], writes=[b0h[0], b0h[1]])
    P.mm(b1[:, 0:N4], ONES128, gflat, reads=[cr, g], writes=[b1h[0], b1h[1]])
    f3 = lambda ap: ap.rearrange("p (c h) -> p c h", h=4)
    P.act(egc[:], f3(b0[0:64, 0:N4]), AF.Exp, reads=[b0h[0], b0h[1]], writes=[egc])
    P.act(edec[:], f3(b1[:, 0:N4]), AF.Exp, reads=[b1h[0], b1h[1]], writes=[edec])
    P.copy(gcs[:], f3(b0[0:64, 0:N4]), reads=[b0h[0], b0h[1]], writes=[gcs], eng="act")
    P.tt(tmpA[:], f3(b1[0:64, 0:N4]), gcs[:], ALU.subtract, reads=[b1h[0], b1h[1], gcs], writes=[tmpA])
    P.act(kdw[:], tmpA[:], AF.Exp, reads=[tmpA], writes=[kdw])
    P.tt(bge[:], beta[:], egc[:], ALU.mult, reads=[beta, egc], writes=[bge])
    QKV = [P.alloc("QKV" + sfx, 64, [3, 4, 128])] * 2
    ss = P.alloc("gss" + sfx, 64, [8])
    rs = P.alloc("grs" + sfx, 64, [8])
    epsq = P.alloc("epsq" + sfx, 64, [1])
    epsk = P.alloc("epsk" + sfx, 64, [1])
    P.memset(epsq[:], 128.0 * EPS, writes=[epsq])
    P.memset(epsk[:], EPS, writes=[epsk])
    Eb = V(SQ, sqf[:, 0:512].rearrange("p (h t j) -> p h t j", h=4, t=2))
    EQ = P.alloc("EQ" + sfx, 64, [4, 64])
    TMP = V(SQ, sqf[:, 512:1024].rearrange("p (h t j) -> p h t j", h=4, t=2))
    UVs = P.alloc("UVs" + sfx, 64, [4, 128])
    OT = P.alloc("OT" + sfx, 64, [4, 128])
    OT2 = [P.alloc("OT2_%d" % i + sfx, 64, [4, 128]) for i in range(2)]
    QKT = P.r("QKT" + sfx, 128, [8, 64])
    GS = P.r("GS" + sfx, 64, [4, 64])
    GT_ = P.r("GT_" + sfx, 64, [4, 64])
    DG = P.r("DG" + sfx, 64, [4, 64])
    AAT = P.r("AAT" + sfx, 64, [4, 2, 64])
    TW = P.r("TW" + sfx, 64, [4, 2, 64])
    TWh = [T(None, "TWa"), T(None, "TWb")]
    YSh = [T(None, "YSa"), T(None, "YSb")]
    YS = P.r("YS" + sfx, 64, [4, 2, 64])
    QKM = P.r("QKM" + sfx, 64, [4, 64])
    BV = P.r("BV" + sfx, 64, [4, 128])
    BGK = P.r("BGK" + sfx, 64, [4, 128])
    KD = P.r("KD" + sfx, 64, [4, 128])
    WKTs = P.r("WKTs" + sfx, 128, [4, 64])
    UN = P.r("UN" + sfx, 64, [4, 128])
    S = P.r("S" + sfx, 128, [4, 128])
    P.copy(S[:], zt[:, 0:512].rearrange("p (h v) -> p h v", h=4), reads=[zt], writes=[S])
    order = list(range(NC)) if d == 0 else list(range(NC - 1, -1, -1))
    for it, ci in enumerate(order):
        t0 = ci * 64
        qkv = QKV[it % 2]
        P.ld(qkv[:].rearrange("p a h v -> p (a h v)"), C_d[t0:t0 + 64, 0:1536], writes=[qkv])
        qk2 = qkv[:, 0:2].rearrange("p a h v -> p (a h) v")
        P.tt(SQ[:], qk2, qk2, ALU.mult, reads=[qkv], writes=[SQ])
        P.reduce(ss[:], SQ[:], ALU.add, reads=[SQ], writes=[ss])
        P.act(rs[:, 0:4], ss[:, 0:4], AF.Sqrt, bias=epsq[:], scale=128.0, reads=[ss, epsq], writes=[rs])
        P.act(rs[:, 4:8], ss[:, 4:8], AF.Sqrt, bias=epsk[:], scale=1.0, reads=[ss, epsk], writes=[rs])
        P.recip(rs[:], rs[:], reads=[rs], writes=[rs])
        P.tt(QKn[:], qk2, bc(rs[:], 2, 128), ALU.mult, reads=[qkv, rs], writes=[QKn])
        for h8 in range(8):
            P.tr(B[0][:, h8 * 64:(h8 + 1) * 64], QKn[:, h8, :], ident[0:64, 0:64], reads=[QKn, ident], writes=[b0h[0], b0h[1]])
        P.copy(QKT[:], B[0][:].rearrange("p (a i) -> p a i", a=8), reads=[b0h[0], b0h[1]], writes=[QKT], eng="act")
        yield
        for h in range(4):
            P.mm(B[1][0:64, h * 64:(h + 1) * 64], QKT[:, 4 + h, :], QKT[:, 4 + h, :], reads=[QKT], writes=[b1h[0], b1h[1]])
        for h in range(4):
            P.mm(B[1][0:64, 256 + h * 64:256 + (h + 1) * 64], QKT[:, 4 + h, :], QKT[:, h, :], reads=[QKT], writes=[b1h[0], b1h[1]])
        gci = g[:, ci, :]
        P.tt(GS[:], bc(SL, 1, 4), bc(gci, 2, 64), ALU.mult, reads=[cr, g], writes=[GS])
        P.tt(GT_[:], bc(TRI, 1, 4), bc(gci, 2, 64), ALU.mult, reads=[cr, g], writes=[GT_])
        P.tt(DG[:], bc(ID64, 1, 4), bc(lnb[:, ci, :], 2, 64), ALU.mult, reads=[craw, lnb], writes=[DG])
        b2v = B[2][0:64, :].rearrange("p (h t j) -> p h t j", h=4, t=2)
        b3v = B[3][0:64, 0:256].rearrange("p (h j) -> p h j", h=4)
        for h in range(4):
            P.mm(b2v[:, h, 0, :], TRI, GS[:, h, :], True, False, reads=[cr, GS], writes=[B[2]])
            P.mm(b2v[:, h, 0, :], DG[:, h, :], ONES64, False, True, reads=[cr, DG], writes=[B[2]])
            P.mm(b2v[:, h, 1, :], SL, GT_[:, h, :], True, False, reads=[cr, GT_], writes=[B[2]])
            P.mm(b2v[:, h, 1, :], ONES64, DG[:, h, :], False, True, reads=[cr, DG], writes=[B[2]])
            P.mm(b3v[:, h, :], SL, GT_[:, h, :], True, False, reads=[cr, GT_], writes=[B[3]])
            P.mm(b3v[:, h, :], ID64r, NEGM, False, True, reads=[cr], writes=[B[3]])
        P.act(Eb[:], b2v, AF.Exp, reads=[B[2]], writes=[Eb])
        yield
        P.act(EQ[:], b3v, AF.Exp, reads=[B[3]], writes=[EQ])
        kkv = B[1][0:64, 0:256].rearrange("p (h j) -> p h j", h=4)
        P.tt(AAT[:], bc(kkv, 2, 2), Eb[:], ALU.mult, reads=[b1h[0], b1h[1], Eb], writes=[AAT])
        P.tt(QKM[:], B[1][0:64, 256:512].rearrange("p (h j) -> p h j", h=4), EQ[:], ALU.mult,
             reads=[b1h[0], b1h[1], EQ], writes=[QKM])
        yield
        P.tt(TMP[:], AAT[:], bc(MK[:, 0], 1, 4), ALU.mult, reads=[AAT, craw], writes=[TMP])
        P.tt(TW[:], bc(ID2, 1, 4), TMP[:], ALU.subtract, reads=[craw, TMP], writes=[TWh[0], TWh[1]])
        yield
        b4v = B[0][0:64, :].rearrange("p (h t j) -> p h t j", h=4, t=2)
        b5v = B[1][0:64, :].rearrange("p (h t j) -> p h t j", h=4, t=2)
        yb = [B[0], B[1]]
        xb_ = [B[2], B[3]]
        yv = [bk_[0:64, 0:256].rearrange("p (h t j) -> p h t j", h=2, t=2) for bk_ in yb]
        xv_ = [bk_[0:64, 0:256].rearrange("p (h t j) -> p h t j", h=2, t=2) for bk_ in xb_]
        for l in range(1, 6):
            for gp in range(2):
                for hh in range(2):
                    h = 2 * gp + hh
                    P.mm(yv[gp][:, hh, 0, :], AAT[:, h, 1, :], TW[:, h, 0, :], reads=[AAT, TWh[gp]], writes=[yb[gp]])
                    P.mm(yv[gp][:, hh, 1, :], AAT[:, h, 0, :], TW[:, h, 1, :], reads=[AAT, TWh[gp]], writes=[yb[gp]])
            for gp in range(2):
                hs = slice(2 * gp, 2 * gp + 2)
                P.tt(YS[:, hs], yv[gp], bc(MK[:, l], 1, 2), ALU.mult, reads=[yb[gp], craw], writes=[YSh[gp]])
            yield
            for gp in range(2):
                for hh in range(2):
                    h = 2 * gp + hh
                    P.mm(xv_[gp][:, hh, 0, :], TW[:, h, 1, :], YS[:, h, 0, :], reads=[TWh[gp], YSh[gp]], writes=[xb_[gp]])
                    P.mm(xv_[gp][:, hh, 1, :], TW[:, h, 0, :], YS[:, h, 1, :], reads=[TWh[gp], YSh[gp]], writes=[xb_[gp]])
            for gp in range(2):
                hs = slice(2 * gp, 2 * gp + 2)
                P.tt(TW[:, hs], TW[:, hs], xv_[gp], ALU.subtract, reads=[TWh[gp], xb_[gp]], writes=[TWh[gp]])
            yield
        P.tt(BV[:], qkv[:, 2], bc(beta[:, ci, :], 2, 128), ALU.mult, reads=[qkv, beta], writes=[BV])
        P.tt(BGK[:], QKn[:, 4:8, :], bc(bge[:, ci, :], 2, 128), ALU.mult, reads=[QKn, bge], writes=[BGK])
        P.tt(KD[:], QKn[:, 4:8, :], bc(kdw[:, ci, :], 2, 128), ALU.mult, reads=[QKn, kdw], writes=[KD])
        for h in range(4):
            P.mm(B[2][0:64, h * 128:(h + 1) * 128], TW[:, h, 1, :], BV[:, h, :], reads=[TWh[0], TWh[1], BV], writes=[B[2]])
        for h in range(4):
            P.mm(B[3][:, 256 + h * 64:256 + (h + 1) * 64], BGK[:, h, :], TW[:, h, 1, :], reads=[TWh[0], TWh[1], BGK], writes=[B[3]])
        P.copy(UVs[:], B[2][0:64, :].rearrange("p (h v) -> p h v", h=4), reads=[B[2]], writes=[UVs], eng="act")
        P.copy(WKTs[:], B[3][:, 256:512].rearrange("p (h i) -> p h i", h=4), reads=[B[3]], writes=[WKTs], eng="act")
        yield
        for h in range(4):
            P.mm(B[0][0:64, h * 128:(h + 1) * 128], WKTs[:, h, :], S[:, h, :], reads=[WKTs, S], writes=[b0h[0], b0h[1]])
        P.tt(UN[:], UVs[:], B[0][0:64, :].rearrange("p (h v) -> p h v", h=4), ALU.subtract,
             reads=[UVs, b0h[0], b0h[1]], writes=[UN])
        yield
        for h in range(4):
            P.mm(B[1][0:64, h * 128:(h + 1) * 128], QKT[:, h, :], S[:, h, :], reads=[QKT, S], writes=[b1h[0], b1h[1]])
        for h in range(4):
            P.mm(B[3][0:64, h * 128:(h + 1) * 128], QKM[:, h, :], UN[:, h, :], reads=[QKM, UN], writes=[B[3]])
        for h in range(4):
            P.mm(B[2][:, h * 128:(h + 1) * 128], KD[:, h, :], UN[:, h, :], reads=[KD, UN], writes=[B[2]])
        P.tt(OT[:], B[1][0:64, :].rearrange("p (h v) -> p h v", h=4), bc(egc[:, ci, :], 2, 128), ALU.mult,
             reads=[b1h[0], b1h[1], egc], writes=[OT])
        yield
        ot2 = OT2[it % 2]
        P.tt(ot2[:], OT[:], B[3][0:64, :].rearrange("p (h v) -> p h v", h=4), ALU.add,
             reads=[OT, B[3]], writes=[ot2])
        for h in range(4):
            P.stt(S[:, h, :], S[:, h, :], edec[:, ci, h:h + 1], B[2][:, h * 128:(h + 1) * 128], ALU.mult, ALU.add,
                  reads=[S, edec, B[2]], writes=[S])
        P.ld(O_d[t0:t0 + 64, :], ot2[:].rearrange("p h v -> p (h v)"), reads=[ot2])
        yield


def ssd_gen(P, L, d, B, C_d, Y_d, SCraw, par, ident, cview, cr, craw, zt):
    sfx = "_s%d" % d
    NC = L // 64
    TRI = cview(cr, "TRI%d" % d)
    SL = cview(cr, "SL%d" % d)
    NEGM = cview(craw, "NEGM%d" % d)
    ONES128 = cview(cr, "ONES128")
    ID64r = cview(cr, "IDENT64")
    tmpA = P.alloc("stmpA" + sfx, 64, [NC, 8])
    dt_ = P.alloc("dt" + sfx, 64, [NC, 8])
    dtA = P.r("dtA" + sfx, 64, [NC, 8])
    ecs = P.alloc("ecs" + sfx, 64, [NC, 8])
    css = P.alloc("css" + sfx, 64, [NC, 8])
    wst = P.alloc("wst" + sfx, 64, [NC, 8])
    edec = P.alloc("sedec" + sfx, 128, [NC, 8])
    nexpA = P.alloc("snexpA" + sfx, 64, [8])
    NEGM8 = P.r("NEGM8" + sfx, 64, [8, 64])
    P.copy(NEGM8[:], bc(NEGM, 1, 8), reads=[craw], writes=[NEGM8])
    P.tt(dt_[:], SCraw[:, :, 16 + 8 * d:24 + 8 * d], bc(par[:, 16 + 8 * d:24 + 8 * d], 1, NC), ALU.add,
         reads=[SCraw, par], writes=[dt_])
    softplus_(P, dt_, tmpA, dt_[:], [dt_])
    P.act(nexpA[:], par[:, 32 + 8 * d:40 + 8 * d], AF.Exp, reads=[par], writes=[nexpA])
    P.ts(nexpA[:], nexpA[:], -1.0, ALU.mult, reads=[nexpA], writes=[nexpA])
    P.tt(dtA[:], dt_[:], bc(nexpA[:], 1, NC), ALU.mult, reads=[dt_, nexpA], writes=[dtA])
    nh = max(NC // 2, 1)
    parts = [(0, nh), (nh, NC)] if NC >= 2 else [(0, NC)]
    for k, (c0, c1) in enumerate(parts):
        n = (c1 - c0) * 8
        fl = dtA[:, c0:c1, :].rearrange("p c h -> p (c h)")
        f3 = lambda ap: ap.rearrange("p (c h) -> p c h", h=8)
        P.mm(B[k][0:64, 0:n], TRI, fl, reads=[cr, dtA], writes=[B[k]])
        P.mm(B[2 + k][:, 0:n], ONES128, fl, reads=[cr, dtA], writes=[B[2 + k]])
        P.act(ecs[:, c0:c1, :], f3(B[k][0:64, 0:n]), AF.Exp, reads=[B[k]], writes=[ecs])
        P.act(edec[:, c0:c1, :], f3(B[2 + k][:, 0:n]), AF.Exp, reads=[B[2 + k]], writes=[edec])
        P.copy(css[:, c0:c1, :], f3(B[k][0:64, 0:n]), reads=[B[k]], writes=[css], eng="act")
        P.tt(tmpA[:, c0:c1, :], f3(B[2 + k][0:64, 0:n]), css[:, c0:c1, :], ALU.subtract,
             reads=[B[2 + k], css], writes=[tmpA])
    P.act(wst[:], tmpA[:], AF.Exp, reads=[tmpA], writes=[wst])
    XBC = [P.alloc("XBC" + sfx, 64, [768])] * 2
    EL = P.alloc("EL" + sfx, 64, [8, 64])
    YT = P.alloc("YT" + sfx, 64, [8, 64])
    YO = [P.alloc("YO%d" % i + sfx, 64, [8, 64]) for i in range(2)]
    HT = P.alloc("HT" + sfx, 128, [8, 64])
    BCT = P.r("BCT" + sfx, 128, [2, 64])
    Btok = P.r("Btok" + sfx, 64, [128])
    GU = P.r("GU" + sfx, 64, [8, 64])
    GT = P.r("GT" + sfx, 64, [8, 64])
    xdt = P.r("xdt" + sfx, 64, [8, 64])
    xdtw = P.r("xdtw" + sfx, 64, [8, 64])
    HST = P.r("HST" + sfx, 128, [8, 64])
    P.copy(HST[:], zt[:, 0:512].rearrange("p (h q) -> p h q", h=8), reads=[zt], writes=[HST])
    order = list(range(NC)) if d == 0 else list(range(NC - 1, -1, -1))
    fl8 = lambda t: t[:].rearrange("p h i -> p (h i)")
    for it, ci in enumerate(order):
        t0 = ci * 64
        xb = XBC[it % 2]
        P.ld(xb[:], C_d[t0:t0 + 64, 1536:2304], writes=[xb])
        xv = xb[:, 0:512].rearrange("p (h q) -> p h q", h=8)
        P.tr(B[0][:, 0:64], xb[:, 512:640], ident[0:64, 0:64], reads=[xb, ident], writes=[B[0]])
        P.tr(B[0][:, 64:128], xb[:, 640:768], ident[0:64, 0:64], reads=[xb, ident], writes=[B[0]])
        P.copy(BCT[:], B[0][:, 0:128].rearrange("p (a i) -> p a i", a=2), reads=[B[0]], writes=[BCT], eng="act")
        P.copy(Btok[:], xb[:, 512:640], reads=[xb], writes=[Btok], eng="act")
        yield
        P.mm(B[0][0:64, 128:192], BCT[:, 0, :], BCT[:, 1, :], reads=[BCT], writes=[B[0]])
        P.tt(GU[:], bc(TRI, 1, 8), bc(dtA[:, ci, :], 2, 64), ALU.mult, reads=[cr, dtA], writes=[GU])
        P.mm(B[1][0:64, :], SL, fl8(GU), True, False, reads=[cr, GU], writes=[B[1]])
        P.mm(B[1][0:64, :], ID64r, fl8(NEGM8), False, True, reads=[cr, NEGM8], writes=[B[1]])
        P.act(fl8(EL), B[1][0:64, :], AF.Exp, reads=[B[1]], writes=[EL])
        yield
        P.tt(GT[:], EL[:], bc(B[0][0:64, 128:192], 1, 8), ALU.mult, reads=[EL, B[0]], writes=[GT])
        P.tt(xdt[:], xv, bc(dt_[:, ci, :], 2, 64), ALU.mult, reads=[xb, dt_], writes=[xdt])
        P.tt(xdtw[:], xdt[:], bc(wst[:, ci, :], 2, 64), ALU.mult, reads=[xdt, wst], writes=[xdtw])
        yield
        for h in range(8):
            P.mm(B[2][0:64, h * 64:(h + 1) * 64], GT[:, h, :], xdt[:, h, :], reads=[GT, xdt], writes=[B[2]])
        P.mm(B[3][0:64, :], BCT[:, 1, :], fl8(HST), reads=[BCT, HST], writes=[B[3]])
        P.mm(B[1][:, :], Btok[:], fl8(xdtw), reads=[Btok, xdtw], writes=[B[1]])
        v8 = lambda ap: ap.rearrange("p (h q) -> p h q", h=8)
        P.tt(YT[:], v8(B[3][0:64, :]), bc(ecs[:, ci, :], 2, 64), ALU.mult, reads=[B[3], ecs], writes=[YT])
        yield
        yo = YO[it % 2]
        P.tt(yo[:], YT[:], v8(B[2][0:64, :]), ALU.add, reads=[YT, B[2]], writes=[yo])
        P.tt(HT[:], HST[:], bc(edec[:, ci, :], 2, 64), ALU.mult, reads=[HST, edec], writes=[HT])
        P.tt(HST[:], HT[:], v8(B[1][:, :]), ALU.add, reads=[HT, B[1]], writes=[HST])
        P.ld(Y_d[t0:t0 + 64, :], fl8(yo), reads=[yo])
        yield


def phase_outproj(P, L, O_d, Y_d, SG_d, C_d, par_ap, gnw, snw, w_out, P_out, ident):
    mk = P.mark()
    WoB = P.alloc("WoB", 128, [8, 1024], BF16)
    wst = [P.alloc("owst%d" % i, 128, [1024]) for i in range(2)]
    for mc in range(8):
        st = wst[mc % 2]
        P.ld(st[:], w_out[mc * 128:(mc + 1) * 128, :], writes=[st])
        P.copy(WoB[:, mc, :], st[:], reads=[st], writes=[WoB], eng=("act" if mc % 2 else "dve"))
    gnwb = P.alloc("gnwb", 128, [128])
    P.ld(gnwb[:], gnw.partition_broadcast(128), writes=[gnwb])
    snwb = P.alloc("snwb", 128, [512])
    P.ld(snwb[:], snw.partition_broadcast(128), writes=[snwb])
    parb = P.alloc("parb", 128, [64])
    P.ld(parb[:], par_ap.partition_broadcast(128), writes=[parb])
    epsk = P.alloc("oepsk", 128, [1])
    P.memset(epsk[:], EPS, writes=[epsk])
    of_ = [P.alloc("of%d" % i, 128, [4, 128]) for i in range(2)]
    ob_ = [P.alloc("ob%d" % i, 128, [4, 128]) for i in range(2)]
    sg_ = [P.alloc("sg%d" % i, 128, [1024]) for i in range(2)]
    yf_ = [P.alloc("yf%d" % i, 128, [8, 64]) for i in range(2)]
    yb_ = [P.alloc("yb%d" % i, 128, [8, 64]) for i in range(2)]
    xx_ = [P.alloc("xx%d" % i, 128, [8, 64]) for i in range(2)]
    tq = P.alloc("otq", 128, [4, 128])
    ss2 = P.alloc("oss2", 128, [4])
    rs2 = P.alloc("ors2", 128, [4])
    ss3 = P.alloc("oss3", 128, [1])
    rs3 = P.alloc("ors3", 128, [1])
    junk = P.alloc("ojunk", 128, [512])
    mx = [P.alloc("mx%d" % i, 128, [1024]) for i in range(2)]
    mxT = [P.alloc("mxT%d" % i, 128, [8, 128], BF16) for i in range(2)]
    po = [P.alloc("po%d" % i, 128, [1024]) for i in range(2)]
    B = P.banks
    f2 = lambda t: t[:].rearrange("p a b -> p (a b)")
    for tt_ in range(L // 128):
        b = tt_ % 2
        t0 = tt_ * 128
        of, ob, sg, yf, yb, xx, m = of_[b], ob_[b], sg_[b], yf_[b], yb_[b], xx_[b], mx[b]
        P.ld(f2(of), O_d[0][t0:t0 + 128, :], writes=[of])
        P.ld(f2(ob), O_d[1][t0:t0 + 128, :], writes=[ob])
        P.ld(sg[:], SG_d[t0:t0 + 128, :], writes=[sg])
        P.ld(f2(yf), Y_d[0][t0:t0 + 128, :], writes=[yf])
        P.ld(f2(yb), Y_d[1][t0:t0 + 128, :], writes=[yb])
        P.ld(f2(xx), C_d[t0:t0 + 128, 1536:2048], writes=[xx])
        P.tt(of[:], of[:], ob[:], ALU.add, reads=[of, ob], writes=[of])
        P.tt(tq[:], of[:], of[:], ALU.mult, reads=[of], writes=[tq])
        P.reduce(ss2[:], tq[:], ALU.add, reads=[tq], writes=[ss2])
        P.act(rs2[:], ss2[:], AF.Sqrt, bias=epsk[:], scale=1.0 / 128.0, reads=[ss2, epsk], writes=[rs2])
        P.recip(rs2[:], rs2[:], reads=[rs2], writes=[rs2])
        P.tt(of[:], of[:], bc(rs2[:], 2, 128), ALU.mult, reads=[of, rs2], writes=[of])
        sgg = sg[:, 0:512].rearrange("p (h v) -> p h v", h=4)
        P.tt(tq[:], sgg, bc(gnwb[:], 1, 4), ALU.mult, reads=[sg, gnwb], writes=[tq])
        P.tt(m[:, 0:512].rearrange("p (h v) -> p h v", h=4), of[:], tq[:], ALU.mult, reads=[of, tq], writes=[m])
        P.tt(yf[:], yf[:], yb[:], ALU.add, reads=[yf, yb], writes=[yf])
        P.tt(xx[:], xx[:], bc(parb[:, 48:56], 2, 64), ALU.mult, reads=[xx, parb], writes=[xx])
        P.tt(yf[:], yf[:], xx[:], ALU.add, reads=[yf, xx], writes=[yf])
        P.tt(f2(yf), f2(yf), sg[:, 512:1024], ALU.mult, reads=[yf, sg], writes=[yf])
        P.act(junk[:], f2(yf), AF.Square, accum=ss3[:], reads=[yf], writes=[junk, ss3])
        P.act(rs3[:], ss3[:], AF.Sqrt, bias=epsk[:], scale=1.0 / 512.0, reads=[ss3, epsk], writes=[rs3])
        P.recip(rs3[:], rs3[:], reads=[rs3], writes=[rs3])
        P.stt(m[:, 512:1024], f2(yf), rs3[:, 0:1], snwb[:], ALU.mult, ALU.mult, reads=[yf, rs3, snwb], writes=[m])
        for half in range(2):
            bk = B[(tt_ * 2 + half) % 4]
            for k in range(4):
                mc = half * 4 + k
                P.tr(bk[:, k * 128:(k + 1) * 128], m[:, mc * 128:(mc + 1) * 128], ident[:],
                     reads=[m, ident], writes=[bk])
            P.copy(mxT[b][:, half * 4:half * 4 + 4, :], bk[:].rearrange("p (k t) -> p k t", k=4),
                   reads=[bk], writes=[mxT[b]], eng="act")
        for half in range(2):
            bk = B[4 + (tt_ * 2 + half) % 4]
            for mc in range(8):
                P.mm(bk[:, :], mxT[b][:, mc, :], WoB[:, mc, half * 512:(half + 1) * 512], mc == 0, mc == 7,
                     reads=[mxT[b], WoB], writes=[bk])
            P.copy(po[b][:, half * 512:(half + 1) * 512], bk[:, :], reads=[bk], writes=[po[b]], eng="act")
        P.ld(P_out[t0:t0 + 128, :], po[b][:], reads=[po[b]])
    P.release(mk)


def build_mixer(L, na):
    nc = bass.Bass("TRN2", target_bir_lowering=False)
    dtn = nc.dram_tensor
    hin = [dtn("h%d" % i, [L, 1024], F32, kind="ExternalInput").ap() for i in range(na)]
    w_in = dtn("w_in", [1024, NCOLS_IN], F32, kind="ExternalInput").ap()
    cw = dtn("cw", [128, 18, 7], F32, kind="ExternalInput").ap()
    cb = dtn("cb", [128, 18], F32, kind="ExternalInput").ap()
    n1w = dtn("n1w", [128, 8], F32, kind="ExternalInput").ap()
    par_in = dtn("par", [64], F32, kind="ExternalInput").ap()
    gnw = dtn("gnw", [128], F32, kind="ExternalInput").ap()
    snw = dtn("snw", [512], F32, kind="ExternalInput").ap()
    w_out = dtn("w_out", [1024, 1024], F32, kind="ExternalInput").ap()
    consts = dtn("consts", [64, NK], F32, kind="ExternalInput").ap()
    ident_in = dtn("ident", [128, 128], F32, kind="ExternalInput").ap()
    P_out = dtn("P", [L, 1024], F32, kind="ExternalOutput").ap()
    hsum = dtn("hsum", [L, 1024], F32, kind="ExternalOutput").ap() if na > 1 else None
    nT_d = dtn("nT_d", [1024, L], BF16, kind="Internal").ap()
    C_d = dtn("C_d", [L, NCONV], F32, kind="Internal").ap()
    SG_d = dtn("SG_d", [L, 1024], F32, kind="Internal").ap()
    O_d = [dtn("O_d%d" % i, [L, 512], F32, kind="Internal").ap() for i in range(2)]
    Y_d = [dtn("Y_d%d" % i, [L, 512], F32, kind="Internal").ap() for i in range(2)]
    P = Prog(nc, 28250, 15750)
    craw, cr, ident, cview = load_consts(P, consts, ident_in)
    NC = L // 64
    par = P.alloc("par", 64, [64])
    P.ld(par[:], par_in.partition_broadcast(64), writes=[par])
    SCraw = P.alloc("SCraw", 64, [NC, 32])
    zt = P.alloc("zt", 128, [512])
    P.memset(zt[:], 0.0, writes=[zt])
    phase_norm_T(P, L, hin, hsum, nT_d, ident)
    phase_inproj(P, L, nT_d, w_in, n1w, cw, cb, C_d, SG_d, SCraw, ident)
    mk2 = P.mark()
    run_rr([gdn_gen(P, L, d, P.banks[4 * d:4 * d + 4], C_d, O_d[d], SCraw, par, ident, cview, cr, craw, zt)
            for d in range(2)])
    P.release(mk2)
    run_rr([ssd_gen(P, L, d, P.banks[4 * d:4 * d + 4], C_d, Y_d[d], SCraw, par, ident, cview, cr, craw, zt)
            for d in range(2)])
    P.release(mk2)
    phase_outproj(P, L, O_d, Y_d, SG_d, C_d, par_in, gnw, snw, w_out, P_out, ident)
    P.emit()
    return nc


IDENT_NP = np.eye(128, dtype=np.float32)


def mixer_weights(inp, layer, hf):
    w_in = inp["w_in"][layer]
    cols = np.concatenate([
        np.arange(0 + hf * 512, 0 + hf * 512 + 512),
        np.arange(1024 + hf * 512, 1024 + hf * 512 + 512),
        np.arange(2048 + hf * 512, 2048 + hf * 512 + 512),
        np.arange(3072 + hf * 512, 3072 + hf * 512 + 512),
        np.arange(4096 + hf * 128, 4096 + hf * 128 + 128),
        np.arange(4352 + hf * 128, 4352 + hf * 128 + 128),
        np.arange(4608 + hf * 512, 4608 + hf * 512 + 512),
        np.arange(5632 + hf * 512, 5632 + hf * 512 + 512),
        np.arange(6656 + hf * 4, 6656 + hf * 4 + 4),
        np.arange(6664 + hf * 4, 6664 + hf * 4 + 4),
        np.arange(6672 + hf * 4, 6672 + hf * 4 + 4),
        np.arange(6680 + hf * 4, 6680 + hf * 4 + 4),
        np.arange(6688 + hf * 8, 6688 + hf * 8 + 8),
        np.arange(6704 + hf * 8, 6704 + hf * 8 + 8),
    ])
    ccols = cols[:NCONV]
    d = {}
    d["w_in"] = np.ascontiguousarray(w_in[:, cols])
    cwc = inp["conv_w"][layer][:, ccols]
    d["cw"] = np.ascontiguousarray(cwc.T.reshape(18, 128, 7).transpose(1, 0, 2))
    d["cb"] = np.ascontiguousarray(inp["conv_b"][layer][ccols].reshape(18, 128).T)
    d["n1w"] = np.ascontiguousarray(inp["norm1_w"][layer].reshape(8, 128).T)
    par = np.zeros(64, np.float32)
    par[0:8] = inp["gdn_dt_bias"][layer][:, hf * 4:hf * 4 + 4].reshape(-1)
    par[8:16] = inp["gdn_A_log"][layer][:, hf * 4:hf * 4 + 4].reshape(-1)
    par[16:32] = inp["ssd_dt_bias"][layer][:, hf * 8:hf * 8 + 8].reshape(-1)
    par[32:48] = inp["ssd_A_log"][layer][:, hf * 8:hf * 8 + 8].reshape(-1)
    par[48:56] = inp["ssd_D"][layer][hf * 8:hf * 8 + 8]
    d["par"] = par
    d["gnw"] = np.ascontiguousarray(inp["gdn_norm_w"][layer])
    d["snw"] = np.ascontiguousarray(inp["ssd_norm_w"][layer][hf * 512:hf * 512 + 512])
    wo = inp["w_out"][layer]
    d["w_out"] = np.ascontiguousarray(np.concatenate([wo[hf * 512:hf * 512 + 512], wo[1024 + hf * 512:1024 + hf * 512 + 512]], 0))
    d["consts"] = CONSTS
    d["ident"] = IDENT_NP
    return d


def phase_router(P, L, nT_d, w_router, n2w, AFF):
    mk = P.mark()
    TB = 512 if L >= 512 else L
    NQ = TB // 128
    n2 = P.alloc("rn2", 128, [8])
    P.ld(n2[:], n2w[:, :], writes=[n2])
    wr_raw = P.alloc("wr_raw", 128, [8, 16])
    P.ld(wr_raw[:], w_router.rearrange("(dc p) e -> p dc e", p=128), writes=[wr_raw])
    WrB = P.alloc("WrB", 128, [8, 16], BF16)
    P.tt(WrB[:], wr_raw[:], bc(n2[:], 2, 16), ALU.mult, reads=[wr_raw, n2], writes=[WrB])
    nTb = [P.alloc("rnTb%d" % i, 128, [8, TB], BF16) for i in range(2)]
    lgs = P.alloc("lgs", 128, [NQ, 16])
    mx = P.alloc("rmx", 128, [NQ])
    se = P.alloc("rse", 128, [NQ])
    nTv = nT_d.rearrange("(dc p) t -> p dc t", p=128)
    for j in range(L // TB):
        nb = nTb[j % 2]
        P.ld(nb[:], nTv[:, :, j * TB:(j + 1) * TB], writes=[nb])
        bk = P.banks[j % 2]
        for q in range(NQ):
            for dc in range(8):
                P.mm(bk[:, q * 16:(q + 1) * 16], nb[:, dc, q * 128:(q + 1) * 128], WrB[:, dc, :], dc == 0, dc == 7,
                     reads=[nb, WrB], writes=[bk])
        P.copy(lgs[:], bk[:, 0:NQ * 16].rearrange("p (q e) -> p q e", q=NQ), reads=[bk], writes=[lgs], eng="act")
        P.reduce(mx[:], lgs[:], ALU.max, reads=[lgs], writes=[mx])
        P.tt(lgs[:], lgs[:], bc(mx[:], 2, 16), ALU.subtract, reads=[lgs, mx], writes=[lgs])
        P.act(lgs[:], lgs[:], AF.Exp, reads=[lgs], writes=[lgs])
        P.reduce(se[:], lgs[:], ALU.add, reads=[lgs], writes=[se])
        P.recip(se[:], se[:], reads=[se], writes=[se])
        P.tt(AFF[:, j * NQ:(j + 1) * NQ, :], lgs[:], bc(se[:], 2, 16), ALU.mult, reads=[lgs, se], writes=[AFF])
    P.release(mk)


def phase_threshold(P, L, AFF, THRB, cap):
    mk = P.mark()
    NT = L // 128
    ones = P.alloc("tones", 128, [128], BF16)
    P.memset(ones[:], 1.0, writes=[ones])
    gq = P.alloc("gq", 128, [NT, 8], BF16)
    lo = THRB
    hi = P.alloc("hi", 128, [8])
    mid = P.alloc("mid", 128, [8])
    cnt = P.alloc("cnt", 128, [8])
    ge = P.alloc("ge", 128, [8])
    dl = P.alloc("dl", 128, [8])
    P.memset(lo[:], 0.0, writes=[lo])
    P.memset(hi[:], 2.0, writes=[hi])
    bk = P.banks[2]
    NCH = (NT * 8 + 511) // 512
    per = NT // NCH
    for it in range(36):
        P.tt(mid[:], lo[:], hi[:], ALU.add, reads=[lo, hi], writes=[mid])
        P.ts(mid[:], mid[:], 0.5, ALU.mult, reads=[mid], writes=[mid])
        P.tt(gq[:], AFF[:, :, 0:8], bc(mid[:], 1, NT), ALU.is_ge, reads=[AFF, mid], writes=[gq])
        for c in range(NCH):
            P.mm(bk[:, 0:per * 8], ones[:], gq[:, c * per:(c + 1) * per, :].rearrange("p n e -> p (n e)"),
                 c == 0, c == NCH - 1, reads=[ones, gq], writes=[bk])
        P.reduce(cnt[:], bk[:, 0:per * 8].rearrange("p (n e) -> p e n", e=8), ALU.add, reads=[bk], writes=[cnt])
        P.ts(ge[:], cnt[:], float(cap) - 0.5, ALU.is_ge, reads=[cnt], writes=[ge])
        P.tt(dl[:], mid[:], lo[:], ALU.subtract, reads=[mid, lo], writes=[dl])
        P.tt(dl[:], dl[:], ge[:], ALU.mult, reads=[dl, ge], writes=[dl])
        P.tt(lo[:], lo[:], dl[:], ALU.add, reads=[lo, dl], writes=[lo])
        P.tt(dl[:], hi[:], mid[:], ALU.subtract, reads=[hi, mid], writes=[dl])
        P.tt(dl[:], dl[:], ge[:], ALU.mult, reads=[dl, ge], writes=[dl])
        P.tt(hi[:], mid[:], dl[:], ALU.add, reads=[mid, dl], writes=[hi])
    P.release(mk)


def phase_moe_dense(P, L, nT_d, AFF, THRB, n2w, w_gate, w_up, w_down, M_out):
    mk = P.mark()
    SB = 1024 if L >= 1024 else L
    TB = 512
    NSB = L // SB
    NBL = SB // TB
    n2 = P.alloc("mn2", 128, [8])
    P.ld(n2[:], n2w[:, :], writes=[n2])
    acc = P.alloc("macc", 128, [SB // 128, 1024])
    Wg = [P.alloc("Wg%d" % i, 128, [8, 512], BF16) for i in range(2)]
    Wu = [P.alloc("Wu%d" % i, 128, [8, 512], BF16) for i in range(2)]
    Wd = [P.alloc("Wd%d" % i, 128, [4, 1024], BF16) for i in range(2)]
    stg = [P.alloc("stg%d" % i, 128, [2048]) for i in range(2)]
    nTb = [P.alloc("mnTb%d" % i, 128, [8, TB], BF16) for i in range(2)]
    hT = [P.alloc("hT%d" % i, 128, [TB], BF16) for i in range(4)]
    sa = [P.alloc("sa%d" % i, 128, [TB]) for i in range(2)]
    gem = P.alloc("gem", 128, [SB // 128, 8])
    wts = P.alloc("wts", 128, [SB // 128, 8])
    nTv = nT_d.rearrange("(dc p) t -> p dc t", p=128)
    B = P.banks
    kst = 0
    kb = 0
    for sbi in range(NSB):
        s0 = sbi * SB
        affb = AFF[:, s0 // 128:(s0 + SB) // 128, 0:8]
        P.tt(gem[:], affb, bc(THRB[:], 1, SB // 128), ALU.is_ge, reads=[AFF, THRB], writes=[gem])
        P.tt(wts[:], gem[:], affb, ALU.mult, reads=[gem, AFF], writes=[wts])
        for e in range(8):
            wb = (sbi * 8 + e) % 2
            for (src, dst, scaled) in ((w_gate, Wg[wb], True), (w_up, Wu[wb], True)):
                for part in range(2):
                    st = stg[kst % 2]
                    kst += 1
                    P.ld(st[:].rearrange("p (dc f) -> p dc f", dc=4),
                         src[e, part * 512:(part + 1) * 512, :].rearrange("(dc p) f -> p dc f", p=128), writes=[st])
                    for dq in range(4):
                        dcx = part * 4 + dq
                        if dq % 2 == 0:
                            P.act(dst[:, dcx, :], st[:, dq * 512:(dq + 1) * 512], AF.Identity, scale=n2[:, dcx:dcx + 1],
                                  reads=[st, n2], writes=[dst])
                        else:
                            P.ts(dst[:, dcx, :], st[:, dq * 512:(dq + 1) * 512], n2[:, dcx:dcx + 1], ALU.mult,
                                 reads=[st, n2], writes=[dst])
            for part in range(2):
                st = stg[kst % 2]
                kst += 1
                P.ld(st[:].rearrange("p (fc d) -> p fc d", fc=2),
                     w_down[e, part * 256:(part + 1) * 256, :].rearrange("(fc p) d -> p fc d", p=128), writes=[st])
                P.copy(Wd[wb][:, part * 2:(part + 1) * 2, :], st[:].rearrange("p (fc d) -> p fc d", fc=2),
                       reads=[st], writes=[Wd[wb]], eng="act")
            for bl in range(NBL):
                nb = nTb[kb % 2]
                kb += 1
                c0 = s0 + bl * TB
                P.ld(nb[:], nTv[:, :, c0:c0 + TB], writes=[nb])
                for fc in range(4):
                    bg = B[(fc % 2) * 2]
                    bu = B[(fc % 2) * 2 + 1]
                    for dc in range(8):
                        P.mm(bg[:, :], Wg[wb][:, dc, fc * 128:(fc + 1) * 128], nb[:, dc, :], dc == 0, dc == 7,
                             reads=[Wg[wb], nb], writes=[bg])
                    for dc in range(8):
                        P.mm(bu[:, :], Wu[wb][:, dc, fc * 128:(fc + 1) * 128], nb[:, dc, :], dc == 0, dc == 7,
                             reads=[Wu[wb], nb], writes=[bu])
                    s_ = sa[fc % 2]
                    P.act(s_[:], bg[:, :], AF.Silu, reads=[bg], writes=[s_])
                    P.tt(hT[fc][:], s_[:], bu[:, :], ALU.mult, reads=[s_, bu], writes=[hT[fc]])
                for q in range(TB // 128):
                    ti = bl * (TB // 128) + q
                    for half in range(2):
                        by = B[4 + (q * 2 + half) % 4]
                        for fc in range(4):
                            P.mm(by[:, :], hT[fc][:, q * 128:(q + 1) * 128], Wd[wb][:, fc, half * 512:(half + 1) * 512],
                                 fc == 0, fc == 3, reads=[hT[fc], Wd[wb]], writes=[by])
                        av = acc[:, ti, half * 512:(half + 1) * 512]
                        if e == 0:
                            P.act(av, by[:, :], AF.Identity, scale=wts[:, ti, e:e + 1], reads=[by, wts], writes=[acc])
                        else:
                            P.stt(av, by[:, :], wts[:, ti, e:e + 1], av, ALU.mult, ALU.add,
                                  reads=[by, wts, acc], writes=[acc])
        P.ld(M_out[s0:s0 + SB, :].rearrange("(n p) d -> p n d", p=128), acc[:], reads=[acc])
    P.release(mk)


def build_moe(L, na):
    nc = bass.Bass("TRN2", target_bir_lowering=False)
    dtn = nc.dram_tensor
    hin = [dtn("h%d" % i, [L, 1024], F32, kind="ExternalInput").ap() for i in range(na)]
    n2w = dtn("n2w", [128, 8], F32, kind="ExternalInput").ap()
    w_router = dtn("w_router", [1024, 16], F32, kind="ExternalInput").ap()
    w_gate = dtn("w_gate", [8, 1024, 512], F32, kind="ExternalInput").ap()
    w_up = dtn("w_up", [8, 1024, 512], F32, kind="ExternalInput").ap()
    w_down = dtn("w_down", [8, 512, 1024], F32, kind="ExternalInput").ap()
    ident_in = dtn("ident", [128, 128], F32, kind="ExternalInput").ap()
    M_out = dtn("M", [L, 1024], F32, kind="ExternalOutput").ap()
    hsum = dtn("hsum", [L, 1024], F32, kind="ExternalOutput").ap()
    nT_d = dtn("nT_d", [1024, L], BF16, kind="Internal").ap()
    P = Prog(nc, 36000, 16)
    ident = P.alloc("ident", 128, [128])
    P.ld(ident[:], ident_in[:, :], writes=[ident])
    AFF = P.alloc("AFF", 128, [L // 128, 16])
    THRB = P.alloc("THRB", 128, [8])
    phase_norm_T(P, L, hin, hsum, nT_d, ident)
    phase_router(P, L, nT_d, w_router, n2w, AFF)
    phase_threshold(P, L, AFF, THRB, 2 * L // 16)
    phase_moe_dense(P, L, nT_d, AFF, THRB, n2w, w_gate, w_up, w_down, M_out)
    P.emit()
    return nc


BONES_NP = np.kron(np.eye(8, dtype=np.float32), np.ones((16, 16), np.float32))


def moe_weights(inp, layer, hf):
    d = {}
    perm = np.concatenate([np.arange(hf * 8, hf * 8 + 8), np.arange((1 - hf) * 8, (1 - hf) * 8 + 8)])
    d["n2w"] = np.ascontiguousarray(inp["norm2_w"][layer].reshape(8, 128).T)
    d["w_router"] = np.ascontiguousarray(inp["w_router"][layer][:, perm])
    d["w_gate"] = np.ascontiguousarray(inp["w_gate"][layer][hf * 8:hf * 8 + 8])
    d["w_up"] = np.ascontiguousarray(inp["w_up"][layer][hf * 8:hf * 8 + 8])
    d["w_down"] = np.ascontiguousarray(inp["w_down"][layer][hf * 8:hf * 8 + 8])
    d["ident"] = IDENT_NP
    return d


def build_final(L, na):
    nc = bass.Bass("TRN2", target_bir_lowering=False)
    dtn = nc.dram_tensor
    hin = [dtn("h%d" % i, [L, 1024], F32, kind="ExternalInput").ap() for i in range(na)]
    fw = dtn("fw", [1024], F32, kind="ExternalInput").ap()
    out = dtn("out", [L, 1024], F32, kind="ExternalOutput").ap()
    P = Prog(nc, 20000, 16)
    fwb = P.alloc("fwb", 128, [1024])
    P.ld(fwb[:], fw.partition_broadcast(128), writes=[fwb])
    hb = [[P.alloc("hb%d_%d" % (i, j), 128, [1024]) for j in range(na)] for i in range(2)]
    junk = P.alloc("junk", 128, [1024])
    ob = [P.alloc("ob%d" % i, 128, [1024]) for i in range(2)]
    ss = [P.alloc("ss%d" % i, 128, [1]) for i in range(2)]
    rs = [P.alloc("rs%d" % i, 128, [1]) for i in range(2)]
    epst = P.alloc("epst", 128, [1])
    P.memset(epst[:], EPS, writes=[epst])
    for tt_ in range(L // 128):
        b = tt_ % 2
        t0 = tt_ * 128
        for j in range(na):
            P.ld(hb[b][j][:], hin[j][t0:t0 + 128, :], writes=[hb[b][j]])
        h = hb[b][0]
        for j in range(1, na):
            P.tt(h[:], h[:], hb[b][j][:], ALU.add, reads=[h, hb[b][j]], writes=[h])
        P.act(junk[:], h[:], AF.Square, accum=ss[b][:], reads=[h], writes=[junk, ss[b]])
        P.act(rs[b][:], ss[b][:], AF.Sqrt, bias=epst[:], scale=1.0 / 1024.0, reads=[ss[b], epst], writes=[rs[b]])
        P.recip(rs[b][:], rs[b][:], reads=[rs[b]], writes=[rs[b]])
        P.stt(ob[b][:], h[:], rs[b][:, 0:1], fwb[:], ALU.mult, ALU.mult, reads=[h, rs[b], fwb], writes=[ob[b]])
        P.ld(out[t0:t0 + 128, :], ob[b][:], reads=[ob[b]])
    P.emit()
    return nc


_CACHE = {}


def _get(kind, L, na):
    key = (kind, L, na)
    if key not in _CACHE:
        _CACHE[key] = {"mixer": build_mixer, "moe": build_moe, "final": build_final}[kind](L, na)
    return _CACHE[key]


def _run(nc, maps, cores, tag):
    import time, sys
    t = time.time()
    r = run_bass_kernel_spmd(nc, maps, core_ids=cores).results
    print("[kernel] launch %s: %.1fs" % (tag, time.time() - t), file=sys.stderr, flush=True)
    return r


def kernel(**inp):
    inp = {k: np.asarray(v) for k, v in inp.items()}
    x = inp["x"]
    Bn, L, D = x.shape
    cores = list(range(8))
    zeros = np.zeros((L, D), np.float32)
    add = [[np.ascontiguousarray(x[b]), zeros, zeros] for b in range(Bn)]
    for layer in range(DEPTH):
        nc = _get("mixer", L, 3)
        maps = []
        for c in cores:
            b, hf = c // 2, c % 2
            d = mixer_weights(inp, layer, hf)
            for i in range(3):
                d["h%d" % i] = add[b][i]
            maps.append(d)
        res = _run(nc, maps, cores, 'mixer%d' % layer)
        add = [[res[2 * b]["hsum"], res[2 * b]["P"], res[2 * b + 1]["P"]] for b in range(Bn)]
        nc = _get("moe", L, 3)
        maps = []
        for c in cores:
            b, hf = c // 2, c % 2
            d = moe_weights(inp, layer, hf)
            for i in range(3):
                d["h%d" % i] = add[b][i]
            maps.append(d)
        res = _run(nc, maps, cores, 'moe%d' % layer)
        add = [[res[2 * b]["hsum"], res[2 * b]["M"], res[2 * b + 1]["M"]] for b in range(Bn)]
    nc = _get("final", L // 2, 3)
    maps = []
    for c in cores:
        b, hf = c // 2, c % 2
        d = {"fw": np.ascontiguousarray(inp["final_norm_w"])}
        for i in range(3):
            d["h%d" % i] = np.ascontiguousarray(add[b][i][hf * (L // 2):(hf + 1) * (L // 2)])
        maps.append(d)
    res = _run(nc, maps, cores, 'final')
    out = np.stack([np.concatenate([res[2 * b]["out"], res[2 * b + 1]["out"]], 0) for b in range(Bn)], 0)
    return out.astype(np.float32)
```
